# Optimizing a Trainium2 kernel written in Bass

```python
import jax, jax.numpy as jnp
from jax import lax
import numpy as np


D_MODEL = 1024
BATCH = 16
SEQ = 4096
DEPTH = 2

GLA_HEADS = 4
GLA_DK = D_MODEL // (2 * GLA_HEADS)
GLA_DV = D_MODEL // GLA_HEADS
GLA_GATE_RANK = 16
GLA_TAU = 16.0
GLA_CHUNK = 64
MLA_HEADS = 16
MLA_NOPE = 64
MLA_ROPE = 32
MLA_V = D_MODEL // MLA_HEADS
MLA_QK = MLA_NOPE + MLA_ROPE
MLA_Q_RANK = 384
MLA_KV_RANK = 128
ROPE_BASE = 10000.0
Q_BLOCK = 128
N_GROUPS = 8
EXPERTS_PER_GROUP = 4
N_EXPERTS = N_GROUPS * EXPERTS_PER_GROUP
TOP_K_IN_GROUP = 2
D_EXPERT = 256
GLA_QK_W = GLA_HEADS * GLA_DK
GLA_V_W = GLA_HEADS * GLA_DV
MLA_V_W = MLA_HEADS * MLA_V
IN_SPLITS = (GLA_QK_W, GLA_QK_W, GLA_V_W, GLA_V_W, GLA_GATE_RANK, GLA_GATE_RANK,
             MLA_Q_RANK, MLA_KV_RANK, MLA_ROPE, D_MODEL, D_MODEL)
IN_OFFSETS = tuple(int(o) for o in np.cumsum(IN_SPLITS)[:-1])
D_IN = int(sum(IN_SPLITS))
DEEPNORM_ALPHA = (2.0 * DEPTH) ** 0.25
DEEPNORM_BETA = (8.0 * DEPTH) ** -0.25
LN_EPS = 1e-5
RMS_EPS = 1e-6

kernel_name = 'hybrid_gla_mla_hier_moe_deepnorm'


def layer_norm(x, g, b):
    xf = x.astype(jnp.float32)
    mu = jnp.mean(xf, axis=-1, keepdims=True)
    var = jnp.mean(jnp.square(xf - mu), axis=-1, keepdims=True)
    return ((xf - mu) * lax.rsqrt(var + LN_EPS) * g + b).astype(x.dtype)


def rms_norm(x, g):
    xf = x.astype(jnp.float32)
    return (xf * lax.rsqrt(jnp.mean(xf * xf, axis=-1, keepdims=True) + RMS_EPS) * g).astype(x.dtype)


def rope_tables(positions):
    inv = ROPE_BASE ** (-jnp.arange(0, MLA_ROPE, 2, dtype=jnp.float32) / MLA_ROPE)
    ang = positions.astype(jnp.float32)[..., None] * inv
    return jnp.cos(ang), jnp.sin(ang)


def apply_rope(x, cos, sin):
    x1, x2 = jnp.split(x, 2, axis=-1)
    c = cos.astype(x.dtype)
    s = sin.astype(x.dtype)
    return jnp.concatenate([x1 * c - x2 * s, x1 * s + x2 * c], axis=-1)


def gla_chunked(q, k, v, log_a, strict):
    B, S, H, DK = q.shape
    DV = v.shape[-1]
    C = GLA_CHUNK
    N = S // C

    def chunks(t):
        return t.reshape(B, N, C, H, t.shape[-1]).transpose(1, 0, 3, 2, 4)

    qc, kc, vc, gc = chunks(q), chunks(k), chunks(v), chunks(log_a)
    b = jnp.cumsum(gc.astype(jnp.float32), axis=3)
    b_last = b[:, :, :, -1:, :]
    q_dec = qc * jnp.exp(b).astype(q.dtype)
    k_inv = kc * jnp.exp(-b).astype(k.dtype)
    k_to_end = kc * jnp.exp(b_last - b).astype(k.dtype)
    chunk_decay = jnp.exp(b_last[:, :, :, 0, :]).astype(q.dtype)
    scores = jnp.einsum('nbhik,nbhjk->nbhij', q_dec, k_inv)
    mask = jnp.tril(jnp.ones((C, C), dtype=bool), k=-1 if strict else 0)
    o_intra = jnp.einsum('nbhij,nbhjv->nbhiv', jnp.where(mask, scores, 0.0).astype(v.dtype), vc)

    def step(state, inp):
        q_n, k_n, v_n, dec_n = inp
        o_n = jnp.einsum('bhik,bhkv->bhiv', q_n, state)
        state = dec_n[..., None] * state + jnp.einsum('bhjk,bhjv->bhkv', k_n, v_n)
        return state, o_n

    state0 = jnp.zeros((B, H, DK, DV), v.dtype)
    _, o_inter = lax.scan(step, state0, (q_dec, k_to_end, vc, chunk_decay))
    o = o_intra + o_inter
    return o.transpose(1, 0, 3, 2, 4).reshape(B, S, H, DV)


def gla_branch(gq, gk, gv, gr, zf, zb, wa2_f, ba_f, wa2_b, ba_b, norm_g):
    B, S, _ = gq.shape

    def heads(t, d):
        return t.reshape(B, S, GLA_HEADS, d)

    qh = heads(gq, GLA_DK) * (GLA_DK ** -0.5)
    kh = heads(gk, GLA_DK)
    vh = heads(gv, GLA_DV)
    log_af = jax.nn.log_sigmoid((zf @ wa2_f + ba_f).astype(jnp.float32)) / GLA_TAU
    log_ab = jax.nn.log_sigmoid((zb @ wa2_b + ba_b).astype(jnp.float32)) / GLA_TAU
    o_f = gla_chunked(qh, kh, vh, heads(log_af, GLA_DK), strict=False)
    flip = lambda t: jnp.flip(t, axis=1)
    o_b = flip(gla_chunked(flip(qh), flip(kh), flip(vh), flip(heads(log_ab, GLA_DK)), strict=True))
    o = rms_norm(o_f + o_b, norm_g.reshape(GLA_HEADS, GLA_DV))
    return o.reshape(B, S, GLA_V_W) * jax.nn.silu(gr)


def mla_branch(cq, ckv, k_rope, cos, sin, q_norm_g, w_uq, kv_norm_g, w_ukv):
    B, S, _ = cq.shape
    q = (rms_norm(cq, q_norm_g) @ w_uq).reshape(B, S, MLA_HEADS, MLA_QK)
    q_nope = q[..., :MLA_NOPE]
    q_rope = apply_rope(q[..., MLA_NOPE:], cos[:, :, None, :], sin[:, :, None, :])
    kv = (rms_norm(ckv, kv_norm_g) @ w_ukv).reshape(B, S, MLA_HEADS, MLA_NOPE + MLA_V)
    k_nope = kv[..., :MLA_NOPE]
    v = kv[..., MLA_NOPE:]
    k_r = apply_rope(k_rope, cos, sin)
    scale = MLA_QK ** -0.5
    nb = S // Q_BLOCK

    def to_blocks(t):
        return jnp.moveaxis(t.reshape(B, nb, Q_BLOCK, *t.shape[2:]), 1, 0)

    def attend(blk):
        qn, qr = blk
        s = jnp.einsum('bqhd,bkhd->bhqk', qn, k_nope) + jnp.einsum('bqhr,bkr->bhqk', qr, k_r)
        p = jax.nn.softmax(s.astype(jnp.float32) * scale, axis=-1).astype(v.dtype)
        return jnp.einsum('bhqk,bkhd->bqhd', p, v)

    o = lax.map(attend, (to_blocks(q_nope), to_blocks(q_rope)))
    return jnp.moveaxis(o, 0, 1).reshape(B, S, MLA_V_W)


def hybrid_mixer(h, cos, sin, w_in, b_in, wa2_f, ba_f, wa2_b, ba_b, gla_norm_g,
                 q_norm_g, w_uq, kv_norm_g, w_ukv, w_out):
    proj = h @ w_in + b_in
    (gq, gk, gv, gr, zf, zb, cq, ckv, kr, gate_a, gate_b) = jnp.split(proj, IN_OFFSETS, axis=-1)
    o_a = gla_branch(gq, gk, gv, gr, zf, zb, wa2_f, ba_f, wa2_b, ba_b, gla_norm_g)
    o_b = mla_branch(cq, ckv, kr, cos, sin, q_norm_g, w_uq, kv_norm_g, w_ukv)
    merged = jax.nn.sigmoid(gate_a) * o_a + jax.nn.sigmoid(gate_b) * o_b
    return merged @ w_out


def hier_moe(h, w_grp, b_grp, w_exp, b_exp, w_gate, w_up, w_down):
    B, S, _ = h.shape
    grp_logits = (h @ w_grp + b_grp).astype(jnp.float32)
    grp_prob = jax.nn.softmax(grp_logits, axis=-1)
    g_idx = jnp.argmax(grp_logits, axis=-1)
    g_w = jnp.take_along_axis(grp_prob, g_idx[..., None], axis=-1)
    exp_logits = (h @ w_exp + b_exp).astype(jnp.float32).reshape(B, S, N_GROUPS, EXPERTS_PER_GROUP)
    in_grp = jnp.take_along_axis(exp_logits, g_idx[..., None, None], axis=2)[..., 0, :]
    top_val, top_idx = lax.top_k(in_grp, TOP_K_IN_GROUP)
    e_w = jax.nn.softmax(top_val, axis=-1) * g_w
    e_id = g_idx[..., None] * EXPERTS_PER_GROUP + top_idx
    gates = jnp.sum(jax.nn.one_hot(e_id, N_EXPERTS, dtype=jnp.float32) * e_w[..., None], axis=-2)

    def per_sequence(args):
        xs, gs = args
        hid = jax.nn.silu(jnp.einsum('sd,edf->sef', xs, w_gate)) * jnp.einsum('sd,edf->sef', xs, w_up)
        return jnp.einsum('sef,efd->sd', hid * gs[..., None].astype(hid.dtype), w_down)

    return lax.map(per_sequence, (h, gates))


def setup_inputs(seed: int = 0) -> dict:
    key = jax.random.key(seed)
    ks = iter(jax.random.split(key, 32))
    f32 = jnp.float32
    L, D = DEPTH, D_MODEL

    def normal(shape, scale):
        return jax.random.normal(next(ks), shape, f32) * scale

    def gain(shape):
        return 1.0 + normal(shape, 0.02)

    x = jax.random.normal(next(ks), (BATCH, SEQ, D), f32)
    positions = (jnp.arange(SEQ, dtype=jnp.int32)[None, :]
                 + jax.random.randint(next(ks), (BATCH, 1), 0, 1024, dtype=jnp.int32))
    in_col_scale = jnp.ones((D_IN,), f32).at[2 * GLA_QK_W: 2 * GLA_QK_W + GLA_V_W].set(DEEPNORM_BETA)
    ukv_col_scale = jnp.tile(jnp.concatenate([jnp.ones((MLA_NOPE,), f32),
                                              jnp.full((MLA_V,), DEEPNORM_BETA, f32)]), MLA_HEADS)
    return {
        'x': x,
        'positions': positions,
        'ln_emb_g': gain((D,)),
        'ln_emb_b': normal((D,), 0.02),
        'w_in': normal((L, D, D_IN), D ** -0.5) * in_col_scale,
        'b_in': normal((L, D_IN), 0.02),
        'gla_wa2_f': normal((L, GLA_GATE_RANK, GLA_QK_W), GLA_GATE_RANK ** -0.5),
        'gla_ba_f': normal((L, GLA_QK_W), 0.02),
        'gla_wa2_b': normal((L, GLA_GATE_RANK, GLA_QK_W), GLA_GATE_RANK ** -0.5),
        'gla_ba_b': normal((L, GLA_QK_W), 0.02),
        'gla_norm_g': gain((L, GLA_V_W)),
        'mla_q_norm_g': gain((L, MLA_Q_RANK)),
        'mla_w_uq': normal((L, MLA_Q_RANK, MLA_HEADS * MLA_QK), MLA_Q_RANK ** -0.5),
        'mla_kv_norm_g': gain((L, MLA_KV_RANK)),
        'mla_w_ukv': normal((L, MLA_KV_RANK, MLA_HEADS * (MLA_NOPE + MLA_V)), MLA_KV_RANK ** -0.5) * ukv_col_scale,
        'w_out': normal((L, D, D), (D ** -0.5) * DEEPNORM_BETA),
        'ln1_g': gain((L, D)),
        'ln1_b': normal((L, D), 0.02),
        'w_grp': normal((L, D, N_GROUPS), D ** -0.5),
        'b_grp': normal((L, N_GROUPS), 0.01),
        'w_exp': normal((L, D, N_EXPERTS), D ** -0.5),
        'b_exp': normal((L, N_EXPERTS), 0.01),
        'w_gate': normal((L, N_EXPERTS, D, D_EXPERT), D ** -0.5),
        'w_up': normal((L, N_EXPERTS, D, D_EXPERT), D ** -0.5),
        'w_down': normal((L, N_EXPERTS, D_EXPERT, D), (D_EXPERT ** -0.5) * DEEPNORM_BETA),
        'ln2_g': gain((L, D)),
        'ln2_b': normal((L, D), 0.02),
    }


def reference(x, positions, ln_emb_g, ln_emb_b, w_in, b_in, gla_wa2_f, gla_ba_f, gla_wa2_b, gla_ba_b,
              gla_norm_g, mla_q_norm_g, mla_w_uq, mla_kv_norm_g, mla_w_ukv, w_out, ln1_g, ln1_b,
              w_grp, b_grp, w_exp, b_exp, w_gate, w_up, w_down, ln2_g, ln2_b):
    cos, sin = rope_tables(positions)
    h = layer_norm(x, ln_emb_g, ln_emb_b)
    for l in range(DEPTH):
        mix = hybrid_mixer(h, cos, sin, w_in[l], b_in[l], gla_wa2_f[l], gla_ba_f[l], gla_wa2_b[l],
                           gla_ba_b[l], gla_norm_g[l], mla_q_norm_g[l], mla_w_uq[l], mla_kv_norm_g[l],
                           mla_w_ukv[l], w_out[l])
        h = layer_norm(DEEPNORM_ALPHA * h + mix, ln1_g[l], ln1_b[l])
        ffn = hier_moe(h, w_grp[l], b_grp[l], w_exp[l], b_exp[l], w_gate[l], w_up[l], w_down[l])
        h = layer_norm(DEEPNORM_ALPHA * h + ffn, ln2_g[l], ln2_b[l])
    return h
```

```python
from contextlib import ExitStack
import numpy as np
import ml_dtypes
import concourse.bass as bass
import concourse.mybir as mybir
from concourse.bass_utils import run_bass_kernel_spmd

F32 = mybir.dt.float32
BF16 = mybir.dt.bfloat16
I32 = mybir.dt.int32
AF = mybir.ActivationFunctionType
ALU = mybir.AluOpType
AX = mybir.AxisListType

D = 1024
DEPTH = 2
D_IN = 5696
O_GQ, O_GK, O_GV, O_GR, O_ZF, O_ZB, O_CQ, O_CKV, O_KR, O_GA, O_GB = (
    0, 512, 1024, 2048, 3072, 3088, 3104, 3488, 3616, 3648, 4672)
ALPHA = (2.0 * DEPTH) ** 0.25
LN_EPS = 1e-5
RMS_EPS = 1e-6
NE = 32
MLA_SCALE = 96 ** -0.5
TWO_PI = 2.0 * np.pi

ENGS = ("pe", "act", "dve", "pool", "sp")
NDMASEM = 6


class Op:
    __slots__ = ("eng", "fn", "deps", "signal", "dma", "sem", "val", "prev_same_sem")

    def __init__(self, eng, fn, dma):
        self.eng = eng
        self.fn = fn
        self.deps = []
        self.signal = False
        self.dma = dma
        self.sem = None
        self.val = None
        self.prev_same_sem = None


class Prog:
    def __init__(self, nc, es):
        self.nc = nc
        self.engobj = {"pe": nc.tensor, "act": nc.scalar, "dve": nc.vector, "pool": nc.gpsimd, "sp": nc.sync}
        self.csem = {e: es.enter_context(nc.semaphore("c_" + e)) for e in ENGS}
        self.ccnt = {e: 0 for e in ENGS}
        self.dsem = {e: [es.enter_context(nc.semaphore("d_%s%d" % (e, i))) for i in range(NDMASEM)]
                     for e in ("sp", "pool", "act")}
        self.dcnt = {e: [0] * NDMASEM for e in ("sp", "pool", "act")}
        self.dlast = {e: [None] * NDMASEM for e in ("sp", "pool", "act")}
        self.drr = {e: 0 for e in ("sp", "pool", "act")}
        self.waited = {e: {} for e in ENGS}
        self.ops = {e: [] for e in ENGS}
        self.last_w = {}
        self.readers = {}
        self.last_op = {e: None for e in ENGS}

    def add(self, eng, fn, r=(), w=(), dma=False):
        op = Op(eng, fn, dma)
        deps = []
        for k in r:
            deps.extend(self.last_w.get(k, ()))
        for k in w:
            deps.extend(self.last_w.get(k, ()))
            deps.extend(self.readers.get(k, ()))
        seen = set()
        for d in deps:
            if d.eng == eng and not d.dma and eng == "pe":
                continue
            if id(d) in seen:
                continue
            seen.add(id(d))
            op.deps.append(d)
            d.signal = True
        for k in w:
            if self.readers.get(k) or k not in self.last_w:
                self.last_w[k] = [op]
            else:
                lw = self.last_w[k]
                if lw and lw[-1].eng == eng and not dma and not lw[-1].dma:
                    lw[-1] = op
                else:
                    lw.append(op)
            self.readers[k] = []
        for k in r:
            self.readers.setdefault(k, []).append(op)
        if dma:
            i = self.drr[eng]
            self.drr[eng] = (i + 1) % NDMASEM
            op.sem = ("d", eng, i)
            op.prev_same_sem = self.dlast[eng][i]
            self.dlast[eng][i] = op
            op.signal = True
        self.ops[eng].append(op)
        self.last_op[eng] = op
        return op

    def barrier(self):
        targets = [self.last_op[e] for e in ENGS if self.last_op[e] is not None and not self.last_op[e].dma]
        for q in ("sp", "pool", "act"):
            targets.extend(o for o in self.dlast[q] if o is not None)
        for e in ENGS:
            op = Op(e, None, False)
            for t in targets:
                if t.eng == e and not t.dma:
                    continue
                op.deps.append(t)
                t.signal = True
            self.ops[e].append(op)
        self.last_w = {}
        self.readers = {}

    def flush(self):
        nc = self.nc
        for e in ENGS:
            for op in self.ops[e]:
                if op.fn is None:
                    continue
                if op.dma:
                    _, q, i = op.sem
                    self.dcnt[q][i] += 16
                    op.val = self.dcnt[q][i]
                elif op.signal:
                    self.ccnt[e] += 1
                    op.val = self.ccnt[e]
        with nc.Block() as block:
            for e in ENGS:
                ops = self.ops[e]
                if not ops:
                    continue

                def body(eng, e=e, ops=ops):
                    waited = self.waited[e]

                    def wait(d):
                        if d.dma:
                            _, q, i = d.sem
                            key = ("d", q, i)
                            sem = self.dsem[q][i]
                        else:
                            key = ("c", d.eng)
                            sem = self.csem[d.eng]
                        if waited.get(key, 0) >= d.val:
                            return
                        waited[key] = d.val
                        eng.wait_ge(sem, d.val)

                    for op in ops:
                        for d in op.deps:
                            wait(d)
                        if op.fn is None:
                            continue
                        if op.dma and op.prev_same_sem is not None:
                            wait(op.prev_same_sem)
                        ins = op.fn(eng)
                        if op.dma:
                            _, q, i = op.sem
                            ins.then_inc(self.dsem[q][i], 16)
                        elif op.signal:
                            ins.then_inc(self.csem[e], 1)

                getattr(block, {"pe": "tensor", "act": "scalar", "dve": "vector", "pool": "gpsimd", "sp": "sync"}[e])(body)
        self.ops = {e: [] for e in ENGS}

    def mm(self, out, lhsT, rhs, start=True, stop=True, r=(), w=()):
        return self.add("pe", lambda e: e.matmul(out, lhsT=lhsT, rhs=rhs, start=start, stop=stop), r, w)

    def tr(self, out, in_, ident, r=(), w=()):
        return self.add("pe", lambda e: e.transpose(out, in_, ident), r, w)

    def act(self, out, in_, func, bias=None, scale=None, accum=None, r=(), w=()):
        kw = {}
        if bias is not None:
            kw["bias"] = bias
        if scale is not None:
            kw["scale"] = scale
        if accum is not None:
            kw["accum_out"] = accum
        return self.add("act", lambda e: e.activation(out=out, in_=in_, func=func, **kw), r, w)

    def tt(self, eng, out, in0, in1, op, r=(), w=()):
        return self.add(eng, lambda e: e.tensor_tensor(out=out, in0=in0, in1=in1, op=op), r, w)

    def ts(self, eng, out, in0, s1, op0, s2=None, op1=None, accum=None, r=(), w=()):
        kw = {}
        if op1 is not None:
            kw["op1"] = op1
        if accum is not None:
            kw["accum_out"] = accum
        return self.add(eng, lambda e: e.tensor_scalar(out=out, in0=in0, scalar1=s1, scalar2=s2, op0=op0, **kw), r, w)

    def stt(self, out, in0, scalar, in1, op0, op1, r=(), w=()):
        return self.add("dve", lambda e: e.scalar_tensor_tensor(out=out, in0=in0, scalar=scalar, in1=in1, op0=op0, op1=op1), r, w)

    def cp(self, eng, out, in_, r=(), w=()):
        if eng == "act":
            return self.add("act", lambda e: e.copy(out=out, in_=in_), r, w)
        return self.add(eng, lambda e: e.tensor_copy(out=out, in_=in_), r, w)

    def dma(self, q, out, in_, r=(), w=()):
        return self.add(q, lambda e: e.dma_start(out=out, in_=in_), r, w, dma=True)

    def memset(self, eng, ap, val, r=(), w=()):
        return self.add(eng, lambda e: e.memset(ap, val), r, w)


class Rot:
    def __init__(self, items):
        self.items = items
        self.i = 0

    def next(self):
        it = self.items[self.i]
        self.i = (self.i + 1) % len(self.items)
        return it


def build_program(S, NS=2, debug=False, phases=("p0", "p1", "gla", "mla", "p3", "moe"), depth=DEPTH):
    T = NS * S
    NG = T // 512
    nc = bass.Bass("TRN2", target_bir_lowering=False)

    def din(name, shape, dt=F32):
        return nc.dram_tensor(name, list(shape), dt, kind="ExternalInput").ap()

    skind = "ExternalOutput" if debug else "Internal"

    def dscr(name, shape, dt):
        return nc.dram_tensor(name, list(shape), dt, kind=skind).ap()

    I = dict(
        x=din("x", [T, D]), pos=din("pos", [T], I32),
        ln_emb_g=din("ln_emb_g", [D]), ln_emb_b=din("ln_emb_b", [D]),
        w_in=din("w_in", [DEPTH, D, D_IN]), b_in=din("b_in", [DEPTH, D_IN]),
        wa2_f=din("wa2_f", [DEPTH, 16, 512]), ba_f=din("ba_f", [DEPTH, 512]),
        wa2_b=din("wa2_b", [DEPTH, 16, 512]), ba_b=din("ba_b", [DEPTH, 512]),
        gla_norm_g=din("gla_norm_g", [DEPTH, D]), q_norm_g=din("q_norm_g", [DEPTH, 384]),
        w_uq=din("w_uq", [DEPTH, 384, 1536]), kv_norm_g=din("kv_norm_g", [DEPTH, 128]),
        w_ukv=din("w_ukv", [DEPTH, 128, 2048]), w_out=din("w_out", [DEPTH, D, D]),
        ln1_g=din("ln1_g", [DEPTH, D]), ln1_b=din("ln1_b", [DEPTH, D]),
        w_r=din("w_r", [DEPTH, D, 40]), b_r=din("b_r", [DEPTH, 40]),
        w_gate=din("w_gate", [DEPTH, NE, D, 256]), w_up=din("w_up", [DEPTH, NE, D, 256]),
        w_down=din("w_down", [DEPTH, NE, 256, D]),
        ln2_g=din("ln2_g", [DEPTH, D]), ln2_b=din("ln2_b", [DEPTH, D]),
        c_inv=din("c_inv", [128, 1]), c_ident=din("c_ident", [128, 128]),
        c_maskf=din("c_maskf", [128, 128]), c_maskb=din("c_maskb", [128, 128]),
        c_scan=din("c_scan", [128, 1024]), c_sel=din("c_sel", [32, NE * 128]),
        c_vbias=din("c_vbias", [1, 1040]),
    )
    out = nc.dram_tensor("out", [T, D], F32, kind="ExternalOutput").ap()

    SC = dict(
        h=dscr("s_h", [T, D], F32),
        ropeC=dscr("s_ropeC", [128, T], F32), ropeS=dscr("s_ropeS", [128, T], F32),
        qTg=dscr("s_qTg", [4, 128, T], BF16), kTg=dscr("s_kTg", [4, 128, T], BF16),
        laT=dscr("s_laT", [2, 4, 128, T], F32),
        vg=dscr("s_vg", [T, 1024], BF16), gaT=dscr("s_gaT", [1024, T], BF16), sgbT=dscr("s_sgbT", [1024, T], BF16),
        qnT=dscr("s_qnT", [8, 128, T], BF16), qx1=dscr("s_qx1", [2, 128, T], BF16), qx2=dscr("s_qx2", [2, 128, T], BF16),
        knT=dscr("s_knT", [8, 128, T], BF16), krT=dscr("s_krT", [32, T], BF16),
        vm=dscr("s_vm", [T, 1040], BF16),
        maT=dscr("s_maT", [1024, T], BF16), mbT=dscr("s_mbT", [1024, T], BF16),
        h1T=dscr("s_h1T", [1024, T], BF16), gatesT=dscr("s_gatesT", [32, T], BF16),
        yacc=dscr("s_yacc", [T, D], F32),
    )

    with ExitStack() as es:
        P = Prog(nc, es)

        def sb(name, shape, dt, stack=es):
            return stack.enter_context(nc.sbuf_tensor(name, list(shape), dt))

        def ps(name, shape, dt, stack=es):
            return stack.enter_context(nc.psum_tensor(name, list(shape), dt))

        ident_f = sb("ident_f", [128, 128], F32)
        ident_b = sb("ident_b", [128, 128], BF16)
        ones_b = sb("ones_b", [128, 512], BF16)
        ones_f = sb("ones_f", [128, 128], F32)
        P.dma("sp", ident_f[:], I["c_ident"], w=["ident_f"])
        P.cp("dve", ident_b[:], ident_f[:], r=["ident_f"], w=["ident_b"])
        P.memset("pool", ones_b[:], 1.0, w=["ones_b"])
        P.memset("pool", ones_f[:], 1.0, w=["ones_f"])
        P.barrier()

        def ln_rows(Pq, xt, g_bc, b_bc, tmp, stat, mv, keys_in, key_out, out_t):
            for c in range(2):
                Pq.add("dve", lambda e, c=c: e.bn_stats(out=stat[:, c, :], in_=xt[:, c * 512:(c + 1) * 512]),
                       r=keys_in, w=[("stat", id(stat))])
            Pq.add("dve", lambda e: e.bn_aggr(out=mv[:, 0:2], in_=stat[:].rearrange("p a b -> p (a b)")),
                   r=[("stat", id(stat))], w=[("mv", id(mv))])
            Pq.ts("dve", mv[:, 2:3], mv[:, 1:2], LN_EPS, ALU.add, r=[("mv", id(mv))], w=[("mv2", id(mv))])
            Pq.act(mv[:, 3:4], mv[:, 2:3], AF.Ln, r=[("mv2", id(mv))], w=[("mv3", id(mv))])
            Pq.act(mv[:, 4:5], mv[:, 3:4], AF.Exp, scale=-0.5, r=[("mv3", id(mv))], w=[("mv4", id(mv))])
            Pq.ts("dve", tmp[:], xt[:], mv[:, 0:1], ALU.subtract, s2=mv[:, 4:5], op1=ALU.mult,
                  r=list(keys_in) + [("mv4", id(mv)), ("mv", id(mv))], w=[("tmp", id(tmp))])
            Pq.tt("pool", tmp[:], tmp[:], g_bc[:], ALU.mult, r=[("tmp", id(tmp)), "lnconst"], w=[("tmp", id(tmp))])
            Pq.tt("pool", out_t[:], tmp[:], b_bc[:], ALU.add, r=[("tmp", id(tmp)), "lnconst"], w=[key_out])

        if "p0" in phases:
            with ExitStack() as st:
                g_bc = sb("p0_g", [128, D], F32, st)
                b_bc = sb("p0_b", [128, D], F32, st)
                inv = sb("p0_inv", [128, 1], F32, st)
                P.dma("sp", g_bc[:], I["ln_emb_g"].partition_broadcast(128), w=["lnconst"])
                P.dma("sp", b_bc[:], I["ln_emb_b"].partition_broadcast(128), w=["lnconst"])
                P.dma("sp", inv[:], I["c_inv"], w=["inv"])
                NB = 3
                xt = [sb("p0_x%d" % i, [128, D], F32, st) for i in range(NB)]
                tmp = [sb("p0_t%d" % i, [128, D], F32, st) for i in range(NB)]
                ot = [sb("p0_o%d" % i, [128, D], F32, st) for i in range(NB)]
                stat = [sb("p0_s%d" % i, [128, 2, 6], F32, st) for i in range(NB)]
                mv = [sb("p0_m%d" % i, [128, 8], F32, st) for i in range(NB)]
                for i in range(T // 128):
                    b = i % NB
                    P.dma("sp", xt[b][:], I["x"][i * 128:(i + 1) * 128, :], w=[("x", b)])
                    ln_rows(P, xt[b], g_bc, b_bc, tmp[b], stat[b], mv[b], [("x", b)], ("o", b), ot[b])
                    P.dma("pool", SC["h"][i * 128:(i + 1) * 128, :], ot[b][:], r=[("o", b)])
                pi_ = sb("p0_pi", [128, 512], I32, st)
                pf = sb("p0_pf", [128, 512], F32, st)
                a1 = sb("p0_a1", [128, 512], F32, st)
                a2 = sb("p0_a2", [128, 512], F32, st)
                kf = sb("p0_kf", [128, 512], F32, st)
                for g in range(NG):
                    P.dma("sp", pi_[:], I["pos"][g * 512:(g + 1) * 512].partition_broadcast(128), w=["pi"])
                    P.cp("dve", pf[:], pi_[:], r=["pi"], w=["pf"])
                    P.ts("dve", pf[:], pf[:], inv[:, 0:1], ALU.mult, r=["pf", "inv"], w=["pf"])
                    for (dst, shift, a) in ((SC["ropeS"], 0.0, a1), (SC["ropeC"], 0.25, a2)):
                        kk = "ang%d" % int(shift > 0)
                        if shift:
                            P.ts("dve", a[:], pf[:], shift, ALU.add, r=["pf"], w=[kk])
                            src = a
                        else:
                            src = pf
                        P.cp("dve", pi_[:], src[:], r=["pf", kk], w=["pi"])
                        P.cp("dve", kf[:], pi_[:], r=["pi"], w=["kf"])
                        P.tt("dve", a[:], src[:], kf[:], ALU.subtract, r=["pf", kk, "kf"], w=[kk])
                        P.ts("dve", kf[:], a[:], 0.5, ALU.is_gt, r=[kk], w=["kf"])
                        P.tt("dve", a[:], a[:], kf[:], ALU.subtract, r=[kk, "kf"], w=[kk])
                        P.ts("dve", kf[:], a[:], -0.5, ALU.is_lt, r=[kk], w=["kf"])
                        P.tt("dve", a[:], a[:], kf[:], ALU.add, r=[kk, "kf"], w=[kk])
                        P.act(a[:], a[:], AF.Sin, scale=TWO_PI, r=[kk], w=[kk])
                        P.dma("pool", dst[:, g * 512:(g + 1) * 512], a[:], r=[kk])
                P.barrier()
                P.flush()

        finish(nc, P, I, SC, out, S, NS, T, NG, sb, ps, ident_f, ident_b, ones_b, ones_f, phases, depth, debug)
    return nc


class Ctx:
    pass


def finish(nc, P, I, SC, out, S, NS, T, NG, sb, ps, ident_f, ident_b, ones_b, ones_f, phases, depth, debug):
    C = Ctx()
    C.nc, C.P, C.I, C.SC, C.out, C.S, C.NS, C.T, C.NG = nc, P, I, SC, out, S, NS, T, NG
    uid = [0]

    def sbu(name, shape, dt, stack):
        uid[0] += 1
        return sb("%s_u%d" % (name, uid[0]), shape, dt, stack)

    def psu(name, shape, dt, stack):
        uid[0] += 1
        return ps("%s_u%d" % (name, uid[0]), shape, dt, stack)

    C.sb, C.ps, C.ident_f, C.ident_b, C.ones_b, C.ones_f = sbu, psu, ident_f, ident_b, ones_b, ones_f
    C.debug = debug
    C.stq_i = 0
    for l in range(depth):
        if "p1" in phases:
            phase_p1(C, l)
        if "gla" in phases:
            phase_gla(C, l)
        if "mla" in phases:
            phase_mla(C, l)
        if "p3" in phases:
            phase_p3(C, l)
        if "moe" in phases:
            phase_moe(C, l, last=(l == depth - 1))
    P.barrier()
    P.flush()


def stq(C):
    C.stq_i ^= 1
    return "sp" if C.stq_i else "pool"


def col_ap(vec_ap):
    return vec_ap.rearrange("(p o) -> p o", o=1)


def phase_p1(C, l):
    P, nc, I, SC, T, NG = C.P, C.nc, C.I, C.SC, C.T, C.NG
    ident_b, ones_b, ones_f = C.ident_b, C.ones_b, C.ones_f
    FM = []
    for h in range(4):
        FM.append(("gq", h, O_GQ + 128 * h, 128))
    for h in range(4):
        FM.append(("gk", h, O_GK + 128 * h, 128))
    FM.append(("zf", 0, O_ZF, 16))
    FM.append(("zb", 1, O_ZB, 16))
    for c in range(3):
        FM.append(("cq", c, O_CQ + 128 * c, 128))
    FM.append(("ckv", 0, O_CKV, 128))
    FM.append(("kr1", 0, O_KR, 16))
    FM.append(("kr2", 0, O_KR + 16, 16))
    for c in range(8):
        FM.append(("gr", c, O_GR + 128 * c, 128))
        FM.append(("ga", c, O_GA + 128 * c, 128))
    for c in range(8):
        FM.append(("gb", c, O_GB + 128 * c, 128))
    with ExitStack() as st:
        def sb(n, shp, dt, stack=st):
            return C.sb("p1_" + n, shp, dt, stack)

        w_in = sb("w_in", [128, 8, D_IN], BF16)
        bcol = sb("bcol", [128, len(FM)], F32)
        gvb = sb("gvb", [1, 1024], BF16)
        wa2 = sb("wa2", [16, 2, 512], BF16)
        nba = sb("nba", [128, 8], F32)
        gnorm = sb("gnorm", [128, 8], F32)
        qg = sb("qg", [128, 4], F32)
        epsc = sb("epsc", [128, 1], F32)
        w_uq = sb("w_uq", [128, 3, 1536], BF16)
        w_kk = sb("w_kk", [128, 1024], BF16)
        w_vv = sb("w_vv", [128, 16, 65], BF16)
        vbias = sb("vbias", [1, 1040], BF16)
        for k in range(8):
            P.dma("pool", w_in[:, k, :], I["w_in"][l, k * 128:(k + 1) * 128, :], w=["w_in"])
        for ci, (_, _, off, M) in enumerate(FM):
            P.dma("sp", bcol[0:M, ci:ci + 1], col_ap(I["b_in"][l, off:off + M]), w=["bcol"])
        P.dma("pool", gvb[0:1, :], I["b_in"][l:l + 1, O_GV:O_GV + 1024], w=["gvb"])
        P.dma("pool", wa2[:, 0, :], I["wa2_f"][l], w=["wa2"])
        P.dma("pool", wa2[:, 1, :], I["wa2_b"][l], w=["wa2"])
        for d_, nm in enumerate(("ba_f", "ba_b")):
            for h in range(4):
                P.dma("sp", nba[:, d_ * 4 + h:d_ * 4 + h + 1], col_ap(I[nm][l, h * 128:(h + 1) * 128]), w=["nba"])
        P.ts("pool", nba[:], nba[:], -1.0, ALU.mult, r=["nba"], w=["nba"])
        for c in range(8):
            P.dma("sp", gnorm[:, c:c + 1], col_ap(I["gla_norm_g"][l, c * 128:(c + 1) * 128]), w=["gnorm"])
        for k in range(3):
            P.dma("sp", qg[:, k:k + 1], col_ap(I["q_norm_g"][l, k * 128:(k + 1) * 128]), w=["qg"])
        P.dma("sp", qg[:, 3:4], col_ap(I["kv_norm_g"][l, :]), w=["qg"])
        P.memset("pool", epsc[:], RMS_EPS, w=["epsc"])
        P.dma("pool", vbias[0:1, :], I["c_vbias"], w=["vbias"])
        with ExitStack() as st2:
            s_uq = sb("s_uq", [128, 3, 1536], F32, st2)
            s_kv = sb("s_kv", [128, 2048], F32, st2)
            P.dma("sp", s_uq[:], I["w_uq"][l].rearrange("(k p) n -> p k n", p=128), w=["s_uq"])
            P.dma("sp", s_kv[:], I["w_ukv"][l], w=["s_kv"])
            for k in range(3):
                P.ts("dve", w_uq[:, k, :], s_uq[:, k, :], qg[:, k:k + 1], ALU.mult, r=["s_uq", "qg"], w=["w_uq"])
            P.ts("dve", w_kk[:], s_kv[:, 0:1024], qg[:, 3:4], ALU.mult, r=["s_kv", "qg"], w=["w_kk"])
            P.memset("pool", w_vv[:], 0.0, w=["w_vv"])
            P.ts("dve", w_vv[:, :, 0:64], s_kv[:, 1024:2048].rearrange("p (h d) -> p h d", d=64), qg[:, 3:4], ALU.mult,
                 r=["s_kv", "qg"], w=["w_vv"])
            P.barrier()
        w_vvf = w_vv[:].rearrange("p h d -> p (h d)")
        ht = [sb("ht%d" % i, [128, D], F32) for i in range(2)]
        hb = [sb("hb%d" % i, [128, D], BF16) for i in range(2)]
        hT4 = [sb("hT4%d" % i, [128, 8, 512], BF16) for i in range(1)] * 2
        cosT = [sb("cos%d" % i, [128, 512], F32) for i in range(2)]
        sinT = [sb("sin%d" % i, [128, 512], F32) for i in range(2)]
        NFP, NBP = 6, 8
        fpl = [sb("fp%d" % i, [128, 512], F32) for i in range(NFP)]
        bpl = [sb("bp%d" % i, [128, 512], BF16) for i in range(NBP)]
        cq_sb = sb("cq_sb", [128, 3, 512], F32)
        cqn = sb("cqn", [128, 3, 512], BF16)
        ckvn = sb("ckvn", [128, 512], BF16)
        vt = [sb("vt%d" % i, [128, 1040], BF16) for i in range(2)]
        gvt = [sb("gvt%d" % i, [128, 1024], BF16) for i in range(2)]
        zt = [sb("zt%d" % i, [16, 512], BF16) for i in range(2)]
        pst = C.ps("p1_pst", [128, 8, 128], BF16, st)
        NPP = 7
        ppl = [C.ps("p1_pp%d" % i, [128, 512], F32, st) for i in range(NPP)]
        cnt = {"fp": 0, "bp": 0, "pp": 0, "ev": 0}

        def fp():
            i = cnt["fp"] % NFP
            cnt["fp"] += 1
            return fpl[i], ("fp", i)

        def bp():
            i = cnt["bp"] % NBP
            cnt["bp"] += 1
            return bpl[i], ("bp", i)

        def pp():
            i = cnt["pp"] % NPP
            cnt["pp"] += 1
            return ppl[i], ("pp", i)

        def evac_copy(dst, src, r, w):
            cnt["ev"] += 1
            P.cp("act" if cnt["ev"] % 2 else "dve", dst, src, r=r, w=w)

        def rope(f1, k1, f2, k2, cos_, sin_, ck, M, dst1, dst2):
            a, ka = fp()
            b, kb = fp()
            P.tt("pool", a[0:M, :], f1[0:M, :], cos_[0:M, :], ALU.mult, r=[k1, ck], w=[ka])
            P.tt("pool", b[0:M, :], f2[0:M, :], sin_[0:M, :], ALU.mult, r=[k2, ck], w=[kb])
            r1, kr1 = bp()
            P.tt("dve", r1[0:M, :], a[0:M, :], b[0:M, :], ALU.subtract, r=[ka, kb], w=[kr1])
            P.dma(stq(C), dst1, r1[0:M, :], r=[kr1])
            a, ka = fp()
            b, kb = fp()
            P.tt("pool", a[0:M, :], f1[0:M, :], sin_[0:M, :], ALU.mult, r=[k1, ck], w=[ka])
            P.tt("pool", b[0:M, :], f2[0:M, :], cos_[0:M, :], ALU.mult, r=[k2, ck], w=[kb])
            r2, kr2 = bp()
            P.tt("dve", r2[0:M, :], a[0:M, :], b[0:M, :], ALU.add, r=[ka, kb], w=[kr2])
            P.dma(stq(C), dst2, r2[0:M, :], r=[kr2])

        def rms_rstd(sq_list, nfeat):
            pss, kp = pp()
            for i, (sq, ksq) in enumerate(sq_list):
                P.mm(pss[:], ones_b[:, 0:128], sq[:], start=(i == 0), stop=(i == len(sq_list) - 1), r=[ksq, "ones_b"], w=[kp])
            t1, kt1 = fp()
            P.act(t1[:], pss[:], AF.Ln, bias=epsc[:, 0:1], scale=1.0 / nfeat, r=[kp, "epsc"], w=[kt1])
            t2, kt2 = fp()
            P.act(t2[:], t1[:], AF.Exp, scale=-0.5, r=[kt1], w=[kt2])
            return t2, kt2

        for g in range(NG):
            t0 = g * 512
            gb_ = g % 2
            tsl = slice(t0, t0 + 512)
            ck = ("cs", gb_)
            P.dma("sp", cosT[gb_][:], SC["ropeC"][:, tsl], w=[ck])
            P.dma("sp", sinT[gb_][:], SC["ropeS"][:, tsl], w=[ck])
            kh4 = ("hT4", gb_)
            for j in range(4):
                jb = j % 2
                P.dma("sp", ht[jb][:], SC["h"][t0 + j * 128:t0 + (j + 1) * 128, :], w=[("ht", jb)])
                P.cp("act", hb[jb][:], ht[jb][:], r=[("ht", jb)], w=[("hb", jb)])
                for k in range(8):
                    P.tr(pst[:, k, :], hb[jb][:, k * 128:(k + 1) * 128], ident_b[:], r=[("hb", jb), "ident_b"], w=["pst"])
                P.cp("dve", hT4[gb_][:, :, j * 128:(j + 1) * 128], pst[:], r=["pst"], w=[kh4])
            h4 = hT4[gb_]
            hold = {}
            for ci, (kind, idx, off, M) in enumerate(FM):
                pt, kp = pp()
                for k in range(8):
                    P.mm(pt[0:M, :], w_in[:, k, off:off + M], h4[:, k, :], start=(k == 0), stop=(k == 7), r=["w_in", kh4], w=[kp])
                bc = bcol[0:M, ci:ci + 1]
                if kind == "gq":
                    o, ko = bp()
                    P.ts("dve", o[:], pt[:], bc, ALU.add, s2=128 ** -0.5, op1=ALU.mult, r=[kp, "bcol"], w=[ko])
                    P.dma(stq(C), SC["qTg"][idx, :, tsl], o[:], r=[ko])
                elif kind == "gk":
                    o, ko = bp()
                    P.ts("dve", o[:], pt[:], bc, ALU.add, r=[kp, "bcol"], w=[ko])
                    P.dma(stq(C), SC["kTg"][idx, :, tsl], o[:], r=[ko])
                elif kind in ("zf", "zb"):
                    z = zt[idx]
                    kz = ("zt", idx)
                    P.ts("dve", z[:], pt[0:16, :], bc, ALU.add, r=[kp, "bcol"], w=[kz])
                    for h in range(4):
                        p2, kp2 = pp()
                        P.mm(p2[:], wa2[0:16, idx, h * 128:(h + 1) * 128], z[0:16, :], r=["wa2", kz], w=[kp2])
                        e1, ke1 = fp()
                        P.act(e1[:], p2[:], AF.Exp, bias=nba[:, idx * 4 + h:idx * 4 + h + 1], scale=-1.0, r=[kp2, "nba"], w=[ke1])
                        e2, ke2 = fp()
                        P.act(e2[:], e1[:], AF.Ln, bias=ones_f[:, 0:1], r=[ke1, "ones_f"], w=[ke2])
                        P.dma(stq(C), SC["laT"][idx, h, :, tsl], e2[:], r=[ke2])
                elif kind == "cq":
                    P.ts("dve", cq_sb[:, idx, :], pt[:], bc, ALU.add, r=[kp, "bcol"], w=[("cq_sb", idx)])
                    sq, ksq = bp()
                    P.act(sq[:], cq_sb[:, idx, :], AF.Square, r=[("cq_sb", idx)], w=[ksq])
                    hold.setdefault("cqsq", []).append((sq, ksq))
                    if idx == 2:
                        rstd, kr = rms_rstd(hold["cqsq"], 384)
                        for c in range(3):
                            P.tt("dve", cqn[:, c, :], cq_sb[:, c, :], rstd[:], ALU.mult, r=[("cq_sb", c), kr], w=["cqn"])
                        for j in range(8):
                            p2, kp2 = pp()
                            for k in range(3):
                                P.mm(p2[:], w_uq[:, k, j * 128:(j + 1) * 128], cqn[:, k, :], start=(k == 0), stop=(k == 2), r=["w_uq", "cqn"], w=[kp2])
                            o, ko = bp()
                            evac_copy(o[:], p2[:], [kp2], [ko])
                            P.dma(stq(C), SC["qnT"][j, :, tsl], o[:], r=[ko])
                        for c in range(2):
                            fs = []
                            for part in range(2):
                                p2, kp2 = pp()
                                co = 1024 + part * 256 + c * 128
                                for k in range(3):
                                    P.mm(p2[:], w_uq[:, k, co:co + 128], cqn[:, k, :], start=(k == 0), stop=(k == 2), r=["w_uq", "cqn"], w=[kp2])
                                f, kf = fp()
                                P.cp("act", f[:], p2[:], r=[kp2], w=[kf])
                                fs.append((f, kf))
                            rope(fs[0][0], fs[0][1], fs[1][0], fs[1][1], cosT[gb_], sinT[gb_], ck, 128,
                                 SC["qx1"][c, :, tsl], SC["qx2"][c, :, tsl])
                elif kind == "ckv":
                    cs, kcs = fp()
                    P.ts("dve", cs[:], pt[:], bc, ALU.add, r=[kp, "bcol"], w=[kcs])
                    sq, ksq = bp()
                    P.act(sq[:], cs[:], AF.Square, r=[kcs], w=[ksq])
                    rstd, kr = rms_rstd([(sq, ksq)], 128)
                    P.tt("dve", ckvn[:], cs[:], rstd[:], ALU.mult, r=[kcs, kr], w=["ckvn"])
                    for j in range(8):
                        p2, kp2 = pp()
                        P.mm(p2[:], w_kk[:, j * 128:(j + 1) * 128], ckvn[:], r=["w_kk", "ckvn"], w=[kp2])
                        o, ko = bp()
                        evac_copy(o[:], p2[:], [kp2], [ko])
                        P.dma(stq(C), SC["knT"][j, :, tsl], o[:], r=[ko])
                    for j in range(4):
                        jb = j % 2
                        for (c0, c1) in ((0, 455), (455, 910), (910, 1040)):
                            p2, kp2 = pp()
                            wd = c1 - c0
                            P.mm(p2[:, 0:wd], ckvn[:, j * 128:(j + 1) * 128], w_vvf[:, c0:c1], start=True, stop=False, r=["ckvn", "w_vv"], w=[kp2])
                            P.mm(p2[:, 0:wd], ones_b[0:1, 0:128], vbias[0:1, c0:c1], start=False, stop=True, r=["ones_b", "vbias"], w=[kp2])
                            evac_copy(vt[jb][:, c0:c1], p2[:, 0:wd], [kp2], [("vt", jb)])
                        P.dma(stq(C), SC["vm"][t0 + j * 128:t0 + (j + 1) * 128, :], vt[jb][:], r=[("vt", jb)])
                elif kind in ("kr1", "kr2"):
                    f, kf = fp()
                    P.ts("dve", f[0:16, :], pt[0:16, :], bc, ALU.add, r=[kp, "bcol"], w=[kf])
                    hold[kind] = (f, kf)
                    if kind == "kr2":
                        rope(hold["kr1"][0], hold["kr1"][1], f, kf, cosT[gb_], sinT[gb_], ck, 16,
                             SC["krT"][0:16, tsl], SC["krT"][16:32, tsl])
                elif kind == "gr":
                    o, ko = bp()
                    P.act(o[:], pt[:], AF.Silu, bias=bc, r=[kp, "bcol"], w=[ko])
                    hold["gr"] = (o, ko)
                elif kind == "ga":
                    o, ko = bp()
                    P.act(o[:], pt[:], AF.Sigmoid, bias=bc, r=[kp, "bcol"], w=[ko])
                    o2, ko2 = bp()
                    P.stt(o2[:], hold["gr"][0][:], gnorm[:, idx:idx + 1], o[:], ALU.mult, ALU.mult, r=[hold["gr"][1], ko, "gnorm"], w=[ko2])
                    P.dma(stq(C), SC["gaT"][idx * 128:(idx + 1) * 128, tsl], o2[:], r=[ko2])
                elif kind == "gb":
                    o, ko = bp()
                    P.act(o[:], pt[:], AF.Sigmoid, bias=bc, r=[kp, "bcol"], w=[ko])
                    P.dma(stq(C), SC["sgbT"][idx * 128:(idx + 1) * 128, tsl], o[:], r=[ko])
            for j in range(4):
                jb = j % 2
                for n in range(2):
                    p2, kp2 = pp()
                    for k in range(8):
                        P.mm(p2[:], h4[:, k, j * 128:(j + 1) * 128], w_in[:, k, O_GV + n * 512:O_GV + (n + 1) * 512],
                             start=(k == 0), stop=False, r=[kh4, "w_in"], w=[kp2])
                    P.mm(p2[:], ones_b[0:1, 0:128], gvb[0:1, n * 512:(n + 1) * 512], start=False, stop=True, r=["ones_b", "gvb"], w=[kp2])
                    evac_copy(gvt[jb][:, n * 512:(n + 1) * 512], p2[:], [kp2], [("gvt", jb)])
                P.dma(stq(C), SC["vg"][t0 + j * 128:t0 + (j + 1) * 128, :], gvt[jb][:], r=[("gvt", jb)])
        P.barrier()
        with nc.named_scope("p1_l%d" % l):
            P.flush()


def phase_mla(C, l):
    P, nc, I, SC, S, NS = C.P, C.nc, C.I, C.SC, C.S, C.NS
    ones_f = C.ones_f
    NKB = S // 128
    NQ = S // 512
    GW = 2
    NSB = 3
    groups = [list(range(a, min(a + GW, NKB))) for a in range(0, NKB, GW)]
    with ExitStack() as st:
        def sb(n, shp, dt):
            return C.sb("ml_" + n, shp, dt, st)
        V = sb("V", [128, NKB, 1040], BF16)
        kT = [sb("kT%d" % i, [96, S], BF16) for i in range(2)]
        qT = [sb("qT%d" % i, [96, S], BF16) for i in range(2)]
        PT = [sb("PT%d" % i, [128, GW, 512], BF16) for i in range(NSB)]
        OT = [sb("OT%d" % i, [65, 512], F32) for i in range(2)]
        rinv = [sb("rinv%d" % i, [64, 512], F32) for i in range(2)]
        o1 = [sb("o1%d" % i, [64, 512], F32) for i in range(2)]
        sg = [sb("sg%d" % i, [64, 512], BF16) for i in range(2)]
        ob = [sb("ob%d" % i, [64, 512], BF16) for i in range(2)]
        sc = [C.ps("ml_sc%d" % i, [128, GW, 512], F32, st) for i in range(NSB)]
        otp = C.ps("ml_ot", [65, 512], F32, st)
        rbp = C.ps("ml_rb", [64, 512], F32, st)
        heads = [(s, h) for s in range(NS) for h in range(16)]

        def load_head(idx):
            s, h = heads[idx]
            tb = s * S
            hb_ = idx % 2
            kk, kq = ("kT", hb_), ("qT", hb_)
            P.dma("sp", kT[hb_][0:64, :], SC["knT"][h // 2, (h % 2) * 64:(h % 2) * 64 + 64, tb:tb + S], w=[kk])
            P.dma("sp", kT[hb_][64:96, :], SC["krT"][:, tb:tb + S], w=[kk])
            P.dma("sp", qT[hb_][0:64, :], SC["qnT"][h // 2, (h % 2) * 64:(h % 2) * 64 + 64, tb:tb + S], w=[kq])
            P.dma("sp", qT[hb_][64:80, :], SC["qx1"][h // 8, (h % 8) * 16:(h % 8) * 16 + 16, tb:tb + S], w=[kq])
            P.dma("sp", qT[hb_][80:96, :], SC["qx2"][h // 8, (h % 8) * 16:(h % 8) * 16 + 16, tb:tb + S], w=[kq])

        stream = []
        for idx, (s, h) in enumerate(heads):
            for qi in range(NQ):
                for gi, grp in enumerate(groups):
                    stream.append((idx, s, h, qi, gi, grp))

        def S_(n):
            idx, s, h, qi, gi, grp = stream[n]
            hb_ = idx % 2
            b = n % NSB
            if qi == 0 and gi == 0:
                if idx == 0:
                    load_head(0)
                if idx + 1 < len(heads):
                    load_head(idx + 1)
            qsl = slice(qi * 512, (qi + 1) * 512)
            for j, kb in enumerate(grp):
                P.mm(sc[b][:, j, :], kT[hb_][:, kb * 128:(kb + 1) * 128], qT[hb_][:, qsl], r=[("kT", hb_), ("qT", hb_)], w=[("sc", b)])

        def epi1(s, h, qi, ib):
            tb = s * S
            P.dma("sp", sg[ib][:], SC["sgbT"][h * 64:(h + 1) * 64, tb + qi * 512:tb + (qi + 1) * 512], w=[("sg", ib)])
            P.cp("dve", OT[ib][:], otp[:], r=["otp"], w=[("OT", ib)])

        def epi2(s, h, qi, ib):
            tb = s * S
            P.mm(rbp[:], ones_f[64:65, 0:64], OT[ib][64:65, :], r=["ones_f", ("OT", ib)], w=["rbp"])
            P.add("dve", lambda e, ib=ib: e.reciprocal(out=rinv[ib][:], in_=rbp[:]), r=["rbp"], w=[("rinv", ib)])
            P.tt("dve", o1[ib][:], OT[ib][0:64, :], rinv[ib][:], ALU.mult, r=[("OT", ib), ("rinv", ib)], w=[("o1", ib)])
            P.tt("pool", ob[ib][:], o1[ib][:], sg[ib][:], ALU.mult, r=[("o1", ib), ("sg", ib)], w=[("ob", ib)])
            P.dma("pool", SC["mbT"][h * 64:(h + 1) * 64, tb + qi * 512:tb + (qi + 1) * 512], ob[ib][:], r=[("ob", ib)])

        NSTR = len(stream)
        for n in range(min(2, NSTR)):
            S_(n)
        pending = None
        it = 0
        for n in range(NSTR):
            idx, s, h, qi, gi, grp = stream[n]
            b = n % NSB
            ng = len(grp)
            if n + 2 < NSTR:
                S_(n + 2)
            if h == 0 and qi == 0 and gi == 0:
                P.dma("sp", V[:], SC["vm"][s * S:(s + 1) * S, :].rearrange("(n p) d -> p n d", p=128), w=["V"])
            P.act(PT[b][:, 0:ng, :], sc[b][:, 0:ng, :], AF.Exp, scale=MLA_SCALE, r=[("sc", b)], w=[("PT", b)])
            for j, kb in enumerate(grp):
                P.mm(otp[:], V[:, kb, h * 65:(h + 1) * 65], PT[b][:, j, :], start=(kb == 0), stop=(kb == NKB - 1),
                     r=["V", ("PT", b)], w=["otp"])
            if pending is not None:
                epi2(*pending)
                pending = None
            if gi == len(groups) - 1:
                ib = it % 2
                it += 1
                epi1(s, h, qi, ib)
                pending = (s, h, qi, ib)
        if pending is not None:
            epi2(*pending)
        P.barrier()
        with nc.named_scope("mla_l%d" % l):
            P.flush()


def phase_gla(C, l):
    P, nc, I, SC, S, NS = C.P, C.nc, C.I, C.SC, C.S, C.NS
    ident_b, ones_f = C.ident_b, C.ones_f
    NCH = S // 128
    W = min(1024, S)
    NBLK = S // W
    CPB = W // 128
    with ExitStack() as st:
        def sb(n, shp, dt):
            return C.sb("gl_" + n, shp, dt, st)
        maskf = sb("maskf", [128, 128], BF16)
        maskb = sb("maskb", [128, 128], BF16)
        scanm = sb("scanm", [128, 1024], F32)
        epsc = sb("epsc", [128, 1], F32)
        P.dma("pool", maskf[:], I["c_maskf"], w=["maskf"])
        P.dma("pool", maskb[:], I["c_maskb"], w=["maskb"])
        P.dma("sp", scanm[:], I["c_scan"], w=["scanm"])
        P.memset("pool", epsc[:], RMS_EPS, w=["epsc"])
        q = sb("q", [128, S], BF16)
        k = sb("k", [128, S], BF16)
        names = ("qdF", "kiF", "keF", "qdB", "kiB", "keB")
        pre = {n: sb(n, [128, S], BF16) for n in names}
        la = [sb("la%d" % i, [128, W], F32) for i in range(2)]
        bb = [sb("bb%d" % i, [128, W], F32) for i in range(2)]
        tmp = [sb("tmp%d" % i, [128, W], F32) for i in range(3)]
        dec = sb("dec", [128, 2, NCH], F32)
        v = sb("v", [128, NCH, 256], BF16)
        stB = sb("stB", [128, NCH, 256], BF16)
        stf = [sb("stf%d" % i, [128, 256], F32) for i in range(2)]
        stFb = [sb("stFb%d" % i, [128, 256], BF16) for i in range(2)]
        kvall = [sb("kvall%d" % i, [128, NCH, 256], F32) for i in range(2)]
        kEt = [sb("kEt%d" % i, [128, 128], BF16) for i in range(2)]
        t12 = [sb("t12%d" % i, [128, 2, 128], BF16) for i in range(2)]
        junk = [sb("junk%d" % i, [128, 256], F32) for i in range(2)]
        ssq = [sb("ssq%d" % i, [128, 4], F32) for i in range(2)]
        on = [sb("on%d" % i, [128, 256], BF16) for i in range(2)]
        gat = [sb("gat%d" % i, [128, 2, 128], BF16) for i in range(2)]
        mat = [sb("mat%d" % i, [128, 2, 128], BF16) for i in range(2)]
        sTp = [C.ps("gl_sT%d" % i, [128, 4, 128], F32, st) for i in range(2)]
        ops_ = [C.ps("gl_o%d" % i, [128, 512], F32, st) for i in range(2)]
        trp = [C.ps("gl_tr%d" % i, [128, 8, 128], BF16, st) for i in range(2)]
        kvp = [C.ps("gl_kv%d" % i, [128, 512], F32, st) for i in range(2)]
        cnt = {"tmp": 0, "i": 0}

        def tm():
            i = cnt["tmp"] % 3
            cnt["tmp"] += 1
            return tmp[i], ("tmp", i)

        for s in range(NS):
            tb = s * S
            for h in range(4):
                P.dma("sp", q[:], SC["qTg"][h, :, tb:tb + S], w=["q"])
                P.dma("sp", k[:], SC["kTg"][h, :, tb:tb + S], w=["k"])
                P.dma("pool", v[:], SC["vg"][tb:tb + S, h * 256:(h + 1) * 256].rearrange("(n p) d -> p n d", p=128), w=["v"])
                for blk in range(NBLK):
                    bs = slice(blk * W, (blk + 1) * W)
                    for d_ in range(2):
                        P.dma("sp", la[d_][:], SC["laT"][d_, h, :, tb + blk * W:tb + (blk + 1) * W], w=[("la", d_)])
                        P.add("dve", lambda e, d_=d_: e.tensor_tensor_scan(out=bb[d_][:], data0=scanm[:, 0:W], data1=la[d_][:],
                                                                           initial=0.0, op0=ALU.mult, op1=ALU.add),
                              r=[("la", d_), "scanm"], w=[("bb", d_)])
                    bF3 = bb[0][:].rearrange("p (c t) -> p c t", t=128)
                    bB3 = bb[1][:].rearrange("p (c t) -> p c t", t=128)
                    totF = bF3[:, :, 127:128]
                    totB = bB3[:, :, 127:128]
                    P.act(dec[:, 0, blk * CPB:(blk + 1) * CPB], bF3[:, :, 127], AF.Exp, scale=-1.0 / 16.0, r=[("bb", 0)], w=["dec"])
                    P.act(dec[:, 1, blk * CPB:(blk + 1) * CPB], bB3[:, :, 127], AF.Exp, scale=-1.0 / 16.0, r=[("bb", 1)], w=["dec"])

                    def emit(dst, src, e_in, kin, scale=1.0):
                        t, kt = tm()
                        P.act(t[:], e_in, AF.Exp, scale=-scale / 16.0, r=kin, w=[kt])
                        P.tt("dve", pre[dst][:, bs], src[:, bs], t[:], ALU.mult, r=[kt, "q", "k"], w=[dst])
                    emit("qdF", q, bb[0][:], [("bb", 0)])
                    emit("kiF", k, bb[0][:], [("bb", 0)], scale=-1.0)
                    t, kt = tm()
                    P.tt("dve", t[:].rearrange("p (c t) -> p c t", t=128), totF.to_broadcast([128, CPB, 128]), bF3, ALU.subtract,
                         r=[("bb", 0)], w=[kt])
                    emit("keF", k, t[:], [kt])
                    t2, kt2 = tm()
                    P.tt("dve", t2[:].rearrange("p (c t) -> p c t", t=128), totB.to_broadcast([128, CPB, 128]), bB3, ALU.subtract,
                         r=[("bb", 1)], w=[kt2])
                    P.tt("dve", t2[:], t2[:], la[1][:], ALU.add, r=[kt2, ("la", 1)], w=[kt2])
                    emit("qdB", q, t2[:], [kt2])
                    emit("kiB", k, t2[:], [kt2], scale=-1.0)
                    t3, kt3 = tm()
                    P.tt("dve", t3[:], bb[1][:], la[1][:], ALU.subtract, r=[("bb", 1), ("la", 1)], w=[kt3])
                    emit("keB", k, t3[:], [kt3])
                P.memset("pool", stf[0][:], 0.0, w=[("stf", 0)])
                curb = [0]

                def kv_step(d_, nm, c):
                    cs = slice(c * 128, (c + 1) * 128)
                    i = cnt["i"] % 2
                    cnt["i"] += 1
                    P.tr(trp[i][:, 0, :], pre[nm][:, cs], ident_b[:], r=[nm, "ident_b"], w=[("trp", i)])
                    P.cp("act", kEt[i][:], trp[i][:, 0, :], r=[("trp", i)], w=[("kEt", i)])
                    P.mm(kvp[i][:, 0:256], kEt[i][:], v[:, c, :], r=[("kEt", i), "v"], w=[("kvp", i)])
                    P.cp("act", kvall[d_][:, c, :], kvp[i][:, 0:256], r=[("kvp", i)], w=[("kvall", d_, c)])

                def bwd_step(c):
                    cur = curb[0]
                    P.cp("act", stB[:, c, :], stf[cur][:], r=[("stf", cur)], w=[("stB", c)])
                    if c == 0:
                        return
                    P.stt(stf[1 - cur][:], stf[cur][:], dec[:, 1, c:c + 1], kvall[1][:, c, :], ALU.mult, ALU.add,
                          r=[("stf", cur), "dec", ("kvall", 1, c)], w=[("stf", 1 - cur)])
                    curb[0] = 1 - cur

                for c in range(NCH):
                    cb = NCH - 1 - c
                    if cb > 0:
                        kv_step(1, "keB", cb)
                    if c < NCH - 1:
                        kv_step(0, "keF", c)
                    if c >= 1:
                        bwd_step(NCH - c)
                bwd_step(0)
                P.memset("pool", stf[0][:], 0.0, w=[("stf", 0)])
                P.memset("pool", stf[1][:], 0.0, w=[("stf", 1)])
                cur = 0
                for c in range(NCH):
                    cs = slice(c * 128, (c + 1) * 128)
                    i = cnt["i"] % 2
                    cnt["i"] += 1
                    tok0 = tb + c * 128
                    P.cp("act", stFb[i][:], stf[cur][:], r=[("stf", cur)], w=[("stFb", i)])
                    if c < NCH - 1:
                        P.stt(stf[1 - cur][:], stf[cur][:], dec[:, 0, c:c + 1], kvall[0][:, c, :], ALU.mult, ALU.add,
                              r=[("stf", cur), "dec", ("kvall", 0, c)], w=[("stf", 1 - cur)])
                        cur = 1 - cur
                    P.dma("sp", gat[i][:], SC["gaT"][h * 256:(h + 1) * 256, tok0:tok0 + 128].rearrange("(a p) t -> p a t", p=128), w=[("gat", i)])
                    P.mm(sTp[i][:, 0, :], pre["kiF"][:, cs], pre["qdF"][:, cs], r=["kiF", "qdF"], w=[("sTp", i)])
                    P.mm(sTp[i][:, 1, :], pre["kiB"][:, cs], pre["qdB"][:, cs], r=["kiB", "qdB"], w=[("sTp", i)])
                    P.tt("dve", t12[i][:, 0, :], sTp[i][:, 0, :], maskf[:], ALU.mult, r=[("sTp", i), "maskf"], w=[("t12", i)])
                    P.tt("dve", t12[i][:, 1, :], sTp[i][:, 1, :], maskb[:], ALU.mult, r=[("sTp", i), "maskb"], w=[("t12", i)])
                    P.mm(ops_[i][:, 0:256], t12[i][:, 0, :], v[:, c, :], start=True, stop=False, r=[("t12", i), "v"], w=[("ops", i)])
                    P.mm(ops_[i][:, 0:256], t12[i][:, 1, :], v[:, c, :], start=False, stop=False, r=[("t12", i), "v"], w=[("ops", i)])
                    P.mm(ops_[i][:, 0:256], pre["qdF"][:, cs], stFb[i][:], start=False, stop=False, r=["qdF", ("stFb", i)], w=[("ops", i)])
                    P.mm(ops_[i][:, 0:256], pre["qdB"][:, cs], stB[:, c, :], start=False, stop=True, r=["qdB", ("stB", c)], w=[("ops", i)])
                    P.act(junk[i][:], ops_[i][:, 0:256], AF.Square, accum=ssq[i][:, 0:1], r=[("ops", i)], w=[("junk", i), ("ssq", i)])
                    P.act(ssq[i][:, 1:2], ssq[i][:, 0:1], AF.Ln, bias=epsc[:, 0:1], scale=1.0 / 256.0, r=[("ssq", i), "epsc"], w=[("ssq1", i)])
                    P.act(ssq[i][:, 2:3], ssq[i][:, 1:2], AF.Exp, scale=-0.5, r=[("ssq1", i)], w=[("ssq2", i)])
                    P.ts("dve", on[i][:], ops_[i][:, 0:256], ssq[i][:, 2:3], ALU.mult, r=[("ops", i), ("ssq2", i)], w=[("on", i)])
                    for a in range(2):
                        P.tr(trp[i][:, 1 + a, :], on[i][:, a * 128:(a + 1) * 128], ident_b[:], r=[("on", i), "ident_b"], w=[("trp", i)])
                    P.tt("dve", mat[i][:], trp[i][:, 1:3, :], gat[i][:], ALU.mult, r=[("trp", i), ("gat", i)], w=[("mat", i)])
                    P.dma("pool", SC["maT"][h * 256:(h + 1) * 256, tok0:tok0 + 128].rearrange("(a p) t -> p a t", p=128), mat[i][:], r=[("mat", i)])
        P.barrier()
        with nc.named_scope("gla_l%d" % l):
            P.flush()


def ln_rows_g(Pq, xt, g_bc, b_bc, tmp, stat, mv, keys_in, key_out, out_t, tag):
    ks, km, kt = ("stat", tag), ("mv", tag), ("lntmp", tag)
    for c in range(2):
        Pq.add("dve", lambda e, c=c: e.bn_stats(out=stat[:, c, :], in_=xt[:, c * 512:(c + 1) * 512]), r=keys_in, w=[ks])
    Pq.add("dve", lambda e: e.bn_aggr(out=mv[:, 0:2], in_=stat[:].rearrange("p a b -> p (a b)")), r=[ks], w=[km])
    Pq.ts("dve", mv[:, 2:3], mv[:, 1:2], LN_EPS, ALU.add, r=[km], w=[km])
    Pq.act(mv[:, 3:4], mv[:, 2:3], AF.Ln, r=[km], w=[km])
    Pq.act(mv[:, 4:5], mv[:, 3:4], AF.Exp, scale=-0.5, r=[km], w=[km])
    Pq.ts("dve", tmp[:], xt[:], mv[:, 0:1], ALU.subtract, s2=mv[:, 4:5], op1=ALU.mult, r=list(keys_in) + [km], w=[kt])
    Pq.tt("dve", tmp[:], tmp[:], g_bc[:], ALU.mult, r=[kt, "lnconst"], w=[kt])
    Pq.tt("dve", out_t[:], tmp[:], b_bc[:], ALU.add, r=[kt, "lnconst"], w=[key_out])


def phase_p3(C, l):
    P, nc, I, SC, T, NG = C.P, C.nc, C.I, C.SC, C.T, C.NG
    ident_f, ones_f = C.ident_f, C.ones_f
    with ExitStack() as st:
        def sb(n, shp, dt):
            return C.sb("p3_" + n, shp, dt, st)
        w_out = sb("w_out", [128, 8, 1024], BF16)
        w_r = sb("w_r", [128, 8, 40], F32)
        b_r = sb("b_r", [1, 40], F32)
        g_bc = sb("g_bc", [128, D], F32)
        b_bc = sb("b_bc", [128, D], F32)
        P.dma("pool", w_out[:], I["w_out"][l].rearrange("(k p) n -> p k n", p=128), w=["w_out"])
        P.dma("sp", w_r[:], I["w_r"][l].rearrange("(k p) n -> p k n", p=128), w=["w_r"])
        P.dma("sp", b_r[0:1, :], I["b_r"][l:l + 1, :], w=["b_r"])
        P.dma("sp", g_bc[:], I["ln1_g"][l].partition_broadcast(128), w=["lnconst"])
        P.dma("sp", b_bc[:], I["ln1_b"][l].partition_broadcast(128), w=["lnconst"])
        ma = [sb("ma%d" % i, [128, 8, 512], BF16) for i in range(2)]
        mb = [sb("mb%d" % i, [128, 8, 512], BF16) for i in range(2)]
        ht = [sb("ht%d" % i, [128, D], F32) for i in range(2)]
        rt = [sb("rt%d" % i, [128, D], F32) for i in range(2)]
        tmp = [sb("tmp%d" % i, [128, D], F32) for i in range(2)]
        h1 = [sb("h1%d" % i, [128, D], F32) for i in range(2)]
        stat = [sb("stat%d" % i, [128, 2, 6], F32) for i in range(2)]
        mv = [sb("mv%d" % i, [128, 8], F32) for i in range(2)]
        hTf = [sb("hTf%d" % i, [128, 8, 128], F32) for i in range(2)]
        hTb = [sb("hTb%d" % i, [128, 8, 128], BF16) for i in range(2)]
        gs = [sb("gs%d" % i, [128, 256], F32) for i in range(2)]
        gTb = [sb("gTb%d" % i, [32, 128], BF16) for i in range(2)]
        mixp = [C.ps("p3_mix%d" % i, [128, 2, 512], F32, st) for i in range(2)]
        pT = C.ps("p3_pT", [128, 8, 128], F32, st)
        rps = C.ps("p3_rps", [128, 512], F32, st)
        gps = C.ps("p3_gps", [128, 512], F32, st)
        def load_group(g):
            t0 = g * 512
            gb_ = g % 2
            km = ("ma", gb_)
            P.dma("sp", ma[gb_][:], SC["maT"][:, t0:t0 + 512].rearrange("(k p) t -> p k t", p=128), w=[km])
            P.dma("sp", mb[gb_][:], SC["mbT"][:, t0:t0 + 512].rearrange("(k p) t -> p k t", p=128), w=[("mb", gb_)])
            P.tt("dve", ma[gb_][:], ma[gb_][:], mb[gb_][:], ALU.add, r=[km, ("mb", gb_)], w=[km])

        def stage_a(n):
            g, j = divmod(n, 4)
            gb_ = g % 2
            km = ("ma", gb_)
            i = n % 2
            tok0 = n * 128
            if j == 0 and g + 1 < NG:
                load_group(g + 1)
                yield
            P.dma("sp", ht[i][:], SC["h"][tok0:tok0 + 128, :], w=[("ht", i)])
            for nn in range(2):
                for k in range(8):
                    P.mm(mixp[i][:, nn, :], ma[gb_][:, k, j * 128:(j + 1) * 128], w_out[:, k, nn * 512:(nn + 1) * 512],
                         start=(k == 0), stop=(k == 7), r=[km, "w_out"], w=[("mixp", i)])
            yield
            P.stt(rt[i][:], ht[i][:], ALPHA, mixp[i][:].rearrange("p a b -> p (a b)"), ALU.mult, ALU.add,
                  r=[("ht", i), ("mixp", i)], w=[("rt", i)])
            yield
            ks, kmv, kt = ("stat", i), ("mv", i), ("lntmp", i)
            for c in range(2):
                P.add("dve", lambda e, c=c, i=i: e.bn_stats(out=stat[i][:, c, :], in_=rt[i][:, c * 512:(c + 1) * 512]), r=[("rt", i)], w=[ks])
                yield
            P.add("dve", lambda e, i=i: e.bn_aggr(out=mv[i][:, 0:2], in_=stat[i][:].rearrange("p a b -> p (a b)")), r=[ks], w=[kmv])
            yield
            P.ts("dve", mv[i][:, 2:3], mv[i][:, 1:2], LN_EPS, ALU.add, r=[kmv], w=[kmv])
            yield
            P.act(mv[i][:, 3:4], mv[i][:, 2:3], AF.Ln, r=[kmv], w=[kmv])
            P.act(mv[i][:, 4:5], mv[i][:, 3:4], AF.Exp, scale=-0.5, r=[kmv], w=[kmv])
            yield
            P.ts("dve", tmp[i][:], rt[i][:], mv[i][:, 0:1], ALU.subtract, s2=mv[i][:, 4:5], op1=ALU.mult, r=[("rt", i), kmv], w=[kt])
            yield
            P.tt("dve", tmp[i][:], tmp[i][:], g_bc[:], ALU.mult, r=[kt, "lnconst"], w=[kt])
            yield
            P.tt("dve", h1[i][:], tmp[i][:], b_bc[:], ALU.add, r=[kt, "lnconst"], w=[("h1", i)])
            yield
            P.dma("pool", SC["h"][tok0:tok0 + 128, :], h1[i][:], r=[("h1", i)], w=[("ht", i)])
            for k in range(8):
                P.tr(pT[:, k, :], h1[i][:, k * 128:(k + 1) * 128], ident_f[:], r=[("h1", i), "ident_f"], w=["pT"])
            P.cp("act", hTf[i][:], pT[:], r=["pT"], w=[("hTf", i)])
            yield
            P.cp("dve", hTb[i][:], hTf[i][:], r=[("hTf", i)], w=[("hTb", i)])
            P.dma("sp", SC["h1T"][:, tok0:tok0 + 128].rearrange("(k p) t -> p k t", p=128), hTb[i][:], r=[("hTb", i)])
            for k in range(8):
                P.mm(rps[:, 0:40], hTf[i][:, k, :], w_r[:, k, :], start=(k == 0), stop=False, r=[("hTf", i), "w_r"], w=["rps"])
            P.mm(rps[:, 0:40], ones_f[0:1, 0:128], b_r[0:1, :], start=False, stop=True, r=["ones_f", "b_r"], w=["rps"])
            yield
            P.cp("act", gs[i][:, 0:40], rps[:, 0:40], r=["rps"], w=[("gs", i)])
            yield

        def stage_b(n):
            i = n % 2
            tok0 = n * 128
            G = gs[i]
            kg = ("gs", i)
            lg, oh, pen, ml, top8 = G[:, 0:40], G[:, 40:48], G[:, 48:56], G[:, 56:88], G[:, 88:96]
            m1, m2, gates = G[:, 96:128], G[:, 128:160], G[:, 160:192]
            scl = lambda a: G[:, 192 + a:193 + a]
            junk8 = G[:, 208:216]
            P.add("dve", lambda e, G=G: e.tensor_reduce(out=G[:, 192:193], in_=G[:, 0:8], axis=AX.X, op=ALU.max), r=[kg], w=[kg])
            yield
            P.ts("dve", oh, lg[:, 0:8], scl(0), ALU.is_equal, r=[kg], w=[kg])
            yield
            P.ts("dve", scl(1), scl(0), -1.0, ALU.mult, r=[kg], w=[kg])
            yield
            P.act(junk8, lg[:, 0:8], AF.Exp, bias=scl(1), accum=scl(2), r=[kg], w=[kg])
            yield
            P.add("dve", lambda e, G=G: e.reciprocal(out=G[:, 195:196], in_=G[:, 194:195]), r=[kg], w=[kg])
            yield
            P.ts("dve", pen, oh, 1.0, ALU.subtract, s2=1e30, op1=ALU.mult, r=[kg], w=[kg])
            yield
            P.tt("dve", ml.rearrange("p (g e) -> p g e", e=4), lg[:, 8:40].rearrange("p (g e) -> p g e", e=4),
                 pen.rearrange("p (g o) -> p g o", o=1).to_broadcast([128, 8, 4]), ALU.add, r=[kg], w=[kg])
            yield
            P.add("dve", lambda e, G=G: e.max(out=G[:, 88:96], in_=G[:, 56:88]), r=[kg], w=[kg])
            yield
            P.ts("dve", m1, ml, top8[:, 0:1], ALU.is_equal, r=[kg], w=[kg])
            yield
            P.ts("dve", m2, ml, top8[:, 1:2], ALU.is_equal, r=[kg], w=[kg])
            yield
            P.tt("dve", scl(4), top8[:, 0:1], top8[:, 1:2], ALU.subtract, r=[kg], w=[kg])
            yield
            P.act(scl(9), scl(4), AF.Exp, scale=-1.0, r=[kg], w=[kg])
            yield
            P.ts("dve", scl(9), scl(9), 1.0, ALU.add, r=[kg], w=[kg])
            yield
            P.add("dve", lambda e, G=G: e.reciprocal(out=G[:, 197:198], in_=G[:, 201:202]), r=[kg], w=[kg])
            yield
            P.ts("dve", scl(6), scl(5), -1.0, ALU.mult, s2=1.0, op1=ALU.add, r=[kg], w=[kg])
            yield
            P.tt("dve", scl(7), scl(5), scl(3), ALU.mult, r=[kg], w=[kg])
            yield
            P.tt("dve", scl(8), scl(6), scl(3), ALU.mult, r=[kg], w=[kg])
            yield
            P.ts("dve", gates, m1, scl(7), ALU.mult, r=[kg], w=[kg])
            yield
            P.stt(gates, m2, scl(8), gates, ALU.mult, ALU.add, r=[kg], w=[kg])
            yield
            P.tr(gps[0:32, 0:128], gates, ident_f[:], r=[kg, "ident_f"], w=["gps"])
            P.cp("act", gTb[i][:], gps[0:32, 0:128], r=["gps"], w=[("gTb", i)])
            P.dma("pool", SC["gatesT"][:, tok0:tok0 + 128], gTb[i][:], r=[("gTb", i)])
            yield

        NTL = NG * 4
        load_group(0)
        for _ in stage_a(0):
            pass
        for n in range(NTL):
            gens = [stage_b(n)]
            if n + 1 < NTL:
                gens.append(stage_a(n + 1))
            while gens:
                for gnr in list(gens):
                    try:
                        next(gnr)
                    except StopIteration:
                        gens.remove(gnr)
        P.barrier()
        with nc.named_scope("p3_l%d" % l):
            P.flush()


def phase_moe(C, l, last):
    P, nc, I, SC, T = C.P, C.nc, C.I, C.SC, C.T
    TT = 512
    NT = T // TT
    GE = 4
    NPASS = NE // GE
    with ExitStack() as st:
        def sb(n, shp, dt):
            return C.sb("mo_" + n, shp, dt, st)
        wg = [sb("wg%d" % i, [128, GE, 8, 256], BF16) for i in range(2)]
        wu = [sb("wu%d" % i, [128, GE, 8, 256], BF16) for i in range(2)]
        wd = [sb("wd%d" % i, [128, GE, 2, 1024], BF16) for i in range(2)]
        sel = sb("sel", [32, NE * 128], BF16)
        g_bc = sb("g_bc", [128, D], F32)
        b_bc = sb("b_bc", [128, D], F32)
        P.dma("pool", sel[:], I["c_sel"], w=["sel"])
        P.dma("sp", g_bc[:], I["ln2_g"][l].partition_broadcast(128), w=["lnconst"])
        P.dma("sp", b_bc[:], I["ln2_b"][l].partition_broadcast(128), w=["lnconst"])
        hT = [sb("hT%d" % i, [128, 8, TT], BF16) for i in range(2)]
        gT = [sb("gT%d" % i, [32, TT], BF16) for i in range(2)]
        Gs = [sb("Gs%d" % i, [128, GE, TT], BF16) for i in range(2)]
        NR = 3
        sgt = [sb("sg%d" % i, [128, TT], BF16) for i in range(NR)]
        tt_ = [sb("tt%d" % i, [128, TT], BF16) for i in range(NR)]
        hid = [sb("hid%d" % i, [128, GE, 2, TT], BF16) for i in range(2)]
        yt = [sb("yt%d" % i, [128, D], F32) for i in range(2)]
        ya = [sb("ya%d" % i, [128, D], F32) for i in range(2)]
        ht = [sb("ht%d" % i, [128, D], F32) for i in range(2)]
        tmp = [sb("tmp%d" % i, [128, D], F32) for i in range(2)]
        ot = [sb("ot%d" % i, [128, D], F32) for i in range(2)]
        stat = [sb("stat%d" % i, [128, 2, 6], F32) for i in range(2)]
        mv = [sb("mv%d" % i, [128, 8], F32) for i in range(2)]
        psy = [C.ps("mo_y%d" % i, [128, 2, 512], F32, st) for i in range(2)]
        psh = [C.ps("mo_h%d" % i, [128, 512], F32, st) for i in range(4)]
        cnt = {"h": 0, "r": 0, "y": 0}

        def load_w(p):
            wb = p % 2
            for ei in range(GE):
                e = p * GE + ei
                P.dma("pool", wg[wb][:, ei, :, :], I["w_gate"][l, e].rearrange("(k p) f -> p k f", p=128), w=[("wg", wb)])
                P.dma("pool", wu[wb][:, ei, :, :], I["w_up"][l, e].rearrange("(k p) f -> p k f", p=128), w=[("wu", wb)])
                P.dma("pool", wd[wb][:, ei, :, :], I["w_down"][l, e].rearrange("(k p) d -> p k d", p=128), w=[("wd", wb)])

        def nextbank():
            ih = cnt["h"] % 4
            cnt["h"] += 1
            return ih

        def GU(p, t, ei):
            wb = p % 2
            tbf = (p * NT + t) % 2
            tok0 = t * TT
            kh, kgt = ("hT", tbf), ("gT", tbf)
            if ei == 0:
                P.dma("sp", hT[tbf][:], SC["h1T"][:, tok0:tok0 + TT].rearrange("(k p) t -> p k t", p=128), w=[kh])
                P.dma("sp", gT[tbf][:], SC["gatesT"][:, tok0:tok0 + TT], w=[kgt])
                for q in range(GE):
                    ih = nextbank()
                    e = p * GE + q
                    P.mm(psh[ih][:], sel[0:32, e * 128:(e + 1) * 128], gT[tbf][0:32, :], r=["sel", kgt], w=[("psh", ih)])
                    P.cp("act", Gs[tbf][:, q, :], psh[ih][:], r=[("psh", ih)], w=[("Gs", tbf)])
            for fc in range(2):
                ig, iu = nextbank(), nextbank()
                for k in range(8):
                    P.mm(psh[ig][:], wg[wb][:, ei, k, fc * 128:(fc + 1) * 128], hT[tbf][:, k, :], start=(k == 0), stop=(k == 7),
                         r=[("wg", wb), kh], w=[("psh", ig)])
                for k in range(8):
                    P.mm(psh[iu][:], wu[wb][:, ei, k, fc * 128:(fc + 1) * 128], hT[tbf][:, k, :], start=(k == 0), stop=(k == 7),
                         r=[("wu", wb), kh], w=[("psh", iu)])
                ir = cnt["r"] % NR
                cnt["r"] += 1
                P.act(sgt[ir][:], psh[ig][:], AF.Silu, r=[("psh", ig)], w=[("sgt", ir)])
                P.tt("dve", tt_[ir][:], psh[iu][:], sgt[ir][:], ALU.mult, r=[("psh", iu), ("sgt", ir)], w=[("tt", ir)])
                P.tt("pool", hid[tbf][:, ei, fc, :], tt_[ir][:], Gs[tbf][:, ei, :], ALU.mult, r=[("tt", ir), ("Gs", tbf)], w=[("hid", tbf, ei)])

        def DN(p, t, rnd):
            wb = p % 2
            tbf = (p * NT + t) % 2
            tok0 = t * TT + rnd * 256
            for j in range(2):
                c0 = rnd * 256 + j * 128
                for n in range(2):
                    for ei in range(GE):
                        for fc in range(2):
                            P.mm(psy[j][:, n, :], hid[tbf][:, ei, fc, c0:c0 + 128], wd[wb][:, ei, fc, n * 512:(n + 1) * 512],
                                 start=(ei == 0 and fc == 0), stop=(ei == GE - 1 and fc == 1), r=[("hid", tbf, ei), ("wd", wb)], w=[("psy", j)])
            for j in range(2):
                tokj = tok0 + j * 128
                iy = cnt["y"] % 2
                cnt["y"] += 1
                yflat = psy[j][:].rearrange("p a b -> p (a b)")
                if p < NPASS - 1:
                    P.cp("act", yt[iy][:], yflat, r=[("psy", j)], w=[("yt", iy)])
                    if p == 0:
                        P.dma("pool", SC["yacc"][tokj:tokj + 128, :], yt[iy][:], r=[("yt", iy)], w=[("yacc", tokj)])
                    else:
                        P.add("pool", lambda e, tokj=tokj, iy=iy: e.dma_start(out=SC["yacc"][tokj:tokj + 128, :], in_=yt[iy][:], accum_op=ALU.add),
                              r=[("yt", iy)], w=[("yacc", tokj)], dma=True)
                else:
                    P.dma("sp", ya[iy][:], SC["yacc"][tokj:tokj + 128, :], r=[("yacc", tokj)], w=[("ya", iy)])
                    P.tt("dve", yt[iy][:], yflat, ya[iy][:], ALU.add, r=[("psy", j), ("ya", iy)], w=[("yt", iy)])
                    P.dma("sp", ht[iy][:], SC["h"][tokj:tokj + 128, :], w=[("ht", iy)])
                    P.stt(yt[iy][:], ht[iy][:], ALPHA, yt[iy][:], ALU.mult, ALU.add, r=[("ht", iy), ("yt", iy)], w=[("yt", iy)])
                    ln_rows_g(P, yt[iy], g_bc, b_bc, tmp[iy], stat[iy], mv[iy], [("yt", iy)], ("ot", iy), ot[iy], iy)
                    dst = C.out if last else SC["h"]
                    P.dma("sp", dst[tokj:tokj + 128, :], ot[iy][:], r=[("ot", iy)], w=[("ht", iy)])

        load_w(0)
        if NPASS > 1:
            load_w(1)
        pend = []
        for p in range(NPASS):
            for t in range(NT):
                for ei in range(GE):
                    GU(p, t, ei)
                    if pend:
                        pp_, tt2, rnd = pend.pop(0)
                        DN(pp_, tt2, rnd)
                        if not pend and pp_ != p and pp_ + 2 < NPASS:
                            load_w(pp_ + 2)
                while pend:
                    DN(*pend.pop(0))
                pend = [(p, t, 0), (p, t, 1)]
        while pend:
            DN(*pend.pop(0))
        P.barrier()
        with nc.named_scope("moe_l%d" % l):
            P.flush()


def make_consts():
    inv = (10000.0 ** (-np.arange(0, 32, 2, dtype=np.float32) / 32)).astype(np.float32)
    c_inv = (np.tile(inv, 8).reshape(128, 1) / np.float32(2 * np.pi)).astype(np.float32)
    j = np.arange(128)[:, None]
    i = np.arange(128)[None, :]
    c_scan = np.ones((128, 1024), np.float32)
    c_scan[:, ::128] = 0.0
    sel = np.zeros((32, NE * 128), np.float32)
    for e in range(NE):
        sel[e, e * 128:(e + 1) * 128] = 1.0
    vb = np.zeros((1, 1040), np.float32)
    vb[0, 64::65] = 1.0
    return dict(c_inv=c_inv, c_ident=np.eye(128, dtype=np.float32),
                c_maskf=(j <= i).astype(np.float32), c_maskb=(j > i).astype(np.float32),
                c_scan=c_scan, c_sel=sel, c_vbias=vb)


def permute_weights(inp):
    w_uq = np.asarray(inp["mla_w_uq"])
    L = w_uq.shape[0]
    w4 = w_uq.reshape(L, 384, 16, 96)
    w_uq_p = np.concatenate([w4[..., 0:64].reshape(L, 384, 1024), w4[..., 64:80].reshape(L, 384, 256),
                             w4[..., 80:96].reshape(L, 384, 256)], axis=-1)
    w_ukv = np.asarray(inp["mla_w_ukv"]).reshape(L, 128, 16, 128)
    w_ukv_p = np.concatenate([w_ukv[..., 0:64].reshape(L, 128, 1024), w_ukv[..., 64:128].reshape(L, 128, 1024)], axis=-1)
    w_r = np.concatenate([np.asarray(inp["w_grp"]), np.asarray(inp["w_exp"])], axis=-1)
    b_r = np.concatenate([np.asarray(inp["b_grp"]), np.asarray(inp["b_exp"])], axis=-1)
    return dict(w_uq=np.ascontiguousarray(w_uq_p), w_ukv=np.ascontiguousarray(w_ukv_p),
                w_r=np.ascontiguousarray(w_r), b_r=np.ascontiguousarray(b_r))


def make_in_maps(inp, n_cores, NS):
    x = np.asarray(inp["x"])
    pos = np.asarray(inp["positions"])
    B, S, _ = x.shape
    assert B == n_cores * NS
    shared = dict(make_consts())
    shared.update(permute_weights(inp))
    ren = dict(ln_emb_g="ln_emb_g", ln_emb_b="ln_emb_b", w_in="w_in", b_in="b_in", wa2_f="gla_wa2_f", ba_f="gla_ba_f",
               wa2_b="gla_wa2_b", ba_b="gla_ba_b", gla_norm_g="gla_norm_g", q_norm_g="mla_q_norm_g",
               kv_norm_g="mla_kv_norm_g", w_out="w_out", ln1_g="ln1_g", ln1_b="ln1_b", w_gate="w_gate", w_up="w_up",
               w_down="w_down", ln2_g="ln2_g", ln2_b="ln2_b")
    for k, v in ren.items():
        shared[k] = np.ascontiguousarray(np.asarray(inp[v]), dtype=np.float32)
    maps = []
    for c in range(n_cores):
        m = dict(shared)
        m["x"] = np.ascontiguousarray(x[c * NS:(c + 1) * NS].reshape(NS * S, D))
        m["pos"] = np.ascontiguousarray(pos[c * NS:(c + 1) * NS].reshape(NS * S).astype(np.int32))
        maps.append(m)
    return maps


_CACHE = {}


def kernel(**inputs):
    x = np.asarray(inputs["x"])
    B, S, _ = x.shape
    n_cores = 8
    NS = B // n_cores
    key = (S, NS)
    if key not in _CACHE:
        _CACHE[key] = build_program(S, NS)
    nc = _CACHE[key]
    maps = make_in_maps(inputs, n_cores, NS)
    res = run_bass_kernel_spmd(nc, maps, core_ids=list(range(n_cores)))
    outs = [np.asarray(r["out"]).reshape(NS, S, D) for r in res.results]
    return np.concatenate(outs, axis=0).astype(np.float32)
```

```python
from contextlib import ExitStack
import numpy as np
import ml_dtypes
import concourse.bass as bass
import concourse.mybir as mybir
from concourse.bass_utils import run_bass_kernel_spmd

F32 = mybir.dt.float32
BF16 = mybir.dt.bfloat16
I32 = mybir.dt.int32
AF = mybir.ActivationFunctionType
ALU = mybir.AluOpType
AX = mybir.AxisListType

D = 1024
DEPTH = 2
D_IN = 5696
O_GQ, O_GK, O_GV, O_GR, O_ZF, O_ZB, O_CQ, O_CKV, O_KR, O_GA, O_GB = (
    0, 512, 1024, 2048, 3072, 3088, 3104, 3488, 3616, 3648, 4672)
ALPHA = (2.0 * DEPTH) ** 0.25
LN_EPS = 1e-5
RMS_EPS = 1e-6
NE = 32
MLA_SCALE = 96 ** -0.5
TWO_PI = 2.0 * np.pi

ENGS = ("pe", "act", "dve", "pool", "sp")
NDMASEM = 6


class Op:
    __slots__ = ("eng", "fn", "deps", "signal", "dma", "sem", "val", "prev_same_sem")

    def __init__(self, eng, fn, dma):
        self.eng = eng
        self.fn = fn
        self.deps = []
        self.signal = False
        self.dma = dma
        self.sem = None
        self.val = None
        self.prev_same_sem = None


class Prog:
    def __init__(self, nc, es):
        self.nc = nc
        self.engobj = {"pe": nc.tensor, "act": nc.scalar, "dve": nc.vector, "pool": nc.gpsimd, "sp": nc.sync}
        self.csem = {e: es.enter_context(nc.semaphore("c_" + e)) for e in ENGS}
        self.ccnt = {e: 0 for e in ENGS}
        self.dsem = {e: [es.enter_context(nc.semaphore("d_%s%d" % (e, i))) for i in range(NDMASEM)]
                     for e in ("sp", "pool", "act")}
        self.dcnt = {e: [0] * NDMASEM for e in ("sp", "pool", "act")}
        self.dlast = {e: [None] * NDMASEM for e in ("sp", "pool", "act")}
        self.drr = {e: 0 for e in ("sp", "pool", "act")}
        self.waited = {e: {} for e in ENGS}
        self.ops = {e: [] for e in ENGS}
        self.last_w = {}
        self.readers = {}
        self.last_op = {e: None for e in ENGS}

    def add(self, eng, fn, r=(), w=(), dma=False):
        op = Op(eng, fn, dma)
        deps = []
        for k in r:
            deps.extend(self.last_w.get(k, ()))
        for k in w:
            deps.extend(self.last_w.get(k, ()))
            deps.extend(self.readers.get(k, ()))
        seen = set()
        for d in deps:
            if d.eng == eng and not d.dma and eng == "pe":
                continue
            if id(d) in seen:
                continue
            seen.add(id(d))
            op.deps.append(d)
            d.signal = True
        for k in w:
            if self.readers.get(k) or k not in self.last_w:
                self.last_w[k] = [op]
            else:
                lw = self.last_w[k]
                if lw and lw[-1].eng == eng and not dma and not lw[-1].dma:
                    lw[-1] = op
                else:
                    lw.append(op)
            self.readers[k] = []
        for k in r:
            self.readers.setdefault(k, []).append(op)
        if dma:
            i = self.drr[eng]
            self.drr[eng] = (i + 1) % NDMASEM
            op.sem = ("d", eng, i)
            op.prev_same_sem = self.dlast[eng][i]
            self.dlast[eng][i] = op
            op.signal = True
        self.ops[eng].append(op)
        self.last_op[eng] = op
        return op

    def barrier(self):
        targets = [self.last_op[e] for e in ENGS if self.last_op[e] is not None and not self.last_op[e].dma]
        for q in ("sp", "pool", "act"):
            targets.extend(o for o in self.dlast[q] if o is not None)
        for e in ENGS:
            op = Op(e, None, False)
            for t in targets:
                if t.eng == e and not t.dma:
                    continue
                op.deps.append(t)
                t.signal = True
            self.ops[e].append(op)
        self.last_w = {}
        self.readers = {}

    def flush(self):
        nc = self.nc
        for e in ENGS:
            for op in self.ops[e]:
                if op.fn is None:
                    continue
                if op.dma:
                    _, q, i = op.sem
                    self.dcnt[q][i] += 16
                    op.val = self.dcnt[q][i]
                elif op.signal:
                    self.ccnt[e] += 1
                    op.val = self.ccnt[e]
        with nc.Block() as block:
            for e in ENGS:
                ops = self.ops[e]
                if not ops:
                    continue

                def body(eng, e=e, ops=ops):
                    waited = self.waited[e]

                    def wait(d):
                        if d.dma:
                            _, q, i = d.sem
                            key = ("d", q, i)
                            sem = self.dsem[q][i]
                        else:
                            key = ("c", d.eng)
                            sem = self.csem[d.eng]
                        if waited.get(key, 0) >= d.val:
                            return
                        waited[key] = d.val
                        eng.wait_ge(sem, d.val)

                    for op in ops:
                        for d in op.deps:
                            wait(d)
                        if op.fn is None:
                            continue
                        if op.dma and op.prev_same_sem is not None:
                            wait(op.prev_same_sem)
                        ins = op.fn(eng)
                        if op.dma:
                            _, q, i = op.sem
                            ins.then_inc(self.dsem[q][i], 16)
                        elif op.signal:
                            ins.then_inc(self.csem[e], 1)

                getattr(block, {"pe": "tensor", "act": "scalar", "dve": "vector", "pool": "gpsimd", "sp": "sync"}[e])(body)
        self.ops = {e: [] for e in ENGS}

    def mm(self, out, lhsT, rhs, start=True, stop=True, r=(), w=()):
        return self.add("pe", lambda e: e.matmul(out, lhsT=lhsT, rhs=rhs, start=start, stop=stop), r, w)

    def tr(self, out, in_, ident, r=(), w=()):
        return self.add("pe", lambda e: e.transpose(out, in_, ident), r, w)

    def act(self, out, in_, func, bias=None, scale=None, accum=None, r=(), w=()):
        kw = {}
        if bias is not None:
            kw["bias"] = bias
        if scale is not None:
            kw["scale"] = scale
        if accum is not None:
            kw["accum_out"] = accum
        return self.add("act", lambda e: e.activation(out=out, in_=in_, func=func, **kw), r, w)

    def tt(self, eng, out, in0, in1, op, r=(), w=()):
        return self.add(eng, lambda e: e.tensor_tensor(out=out, in0=in0, in1=in1, op=op), r, w)

    def ts(self, eng, out, in0, s1, op0, s2=None, op1=None, accum=None, r=(), w=()):
        kw = {}
        if op1 is not None:
            kw["op1"] = op1
        if accum is not None:
            kw["accum_out"] = accum
        return self.add(eng, lambda e: e.tensor_scalar(out=out, in0=in0, scalar1=s1, scalar2=s2, op0=op0, **kw), r, w)

    def stt(self, out, in0, scalar, in1, op0, op1, r=(), w=()):
        return self.add("dve", lambda e: e.scalar_tensor_tensor(out=out, in0=in0, scalar=scalar, in1=in1, op0=op0, op1=op1), r, w)

    def cp(self, eng, out, in_, r=(), w=()):
        if eng == "act":
            return self.add("act", lambda e: e.copy(out=out, in_=in_), r, w)
        return self.add(eng, lambda e: e.tensor_copy(out=out, in_=in_), r, w)

    def dma(self, q, out, in_, r=(), w=()):
        return self.add(q, lambda e: e.dma_start(out=out, in_=in_), r, w, dma=True)

    def memset(self, eng, ap, val, r=(), w=()):
        return self.add(eng, lambda e: e.memset(ap, val), r, w)


class Rot:
    def __init__(self, items):
        self.items = items
        self.i = 0

    def next(self):
        it = self.items[self.i]
        self.i = (self.i + 1) % len(self.items)
        return it


def build_program(S, NS=2, debug=False, phases=("p0", "p1", "gla", "mla", "p3", "moe"), depth=DEPTH):
    T = NS * S
    NG = T // 512
    nc = bass.Bass("TRN2", target_bir_lowering=False)

    def din(name, shape, dt=F32):
        return nc.dram_tensor(name, list(shape), dt, kind="ExternalInput").ap()

    skind = "ExternalOutput" if debug else "Internal"

    def dscr(name, shape, dt):
        return nc.dram_tensor(name, list(shape), dt, kind=skind).ap()

    I = dict(
        x=din("x", [T, D]), pos=din("pos", [T], I32),
        ln_emb_g=din("ln_emb_g", [D]), ln_emb_b=din("ln_emb_b", [D]),
        w_in=din("w_in", [DEPTH, D, D_IN]), b_in=din("b_in", [DEPTH, D_IN]),
        wa2_f=din("wa2_f", [DEPTH, 16, 512]), ba_f=din("ba_f", [DEPTH, 512]),
        wa2_b=din("wa2_b", [DEPTH, 16, 512]), ba_b=din("ba_b", [DEPTH, 512]),
        gla_norm_g=din("gla_norm_g", [DEPTH, D]), q_norm_g=din("q_norm_g", [DEPTH, 384]),
        w_uq=din("w_uq", [DEPTH, 384, 1536]), kv_norm_g=din("kv_norm_g", [DEPTH, 128]),
        w_ukv=din("w_ukv", [DEPTH, 128, 2048]), w_out=din("w_out", [DEPTH, D, D]),
        ln1_g=din("ln1_g", [DEPTH, D]), ln1_b=din("ln1_b", [DEPTH, D]),
        w_r=din("w_r", [DEPTH, D, 40]), b_r=din("b_r", [DEPTH, 40]),
        w_gate=din("w_gate", [DEPTH, NE, D, 256]), w_up=din("w_up", [DEPTH, NE, D, 256]),
        w_down=din("w_down", [DEPTH, NE, 256, D]),
        ln2_g=din("ln2_g", [DEPTH, D]), ln2_b=din("ln2_b", [DEPTH, D]),
        c_inv=din("c_inv", [128, 1]), c_ident=din("c_ident", [128, 128]),
        c_maskf=din("c_maskf", [128, 128]), c_maskb=din("c_maskb", [128, 128]),
        c_scan=din("c_scan", [128, 1024]), c_sel=din("c_sel", [32, NE * 128]),
        c_vbias=din("c_vbias", [1, 1040]),
    )
    out = nc.dram_tensor("out", [T, D], F32, kind="ExternalOutput").ap()

    SC = dict(
        h=dscr("s_h", [T, D], F32),
        ropeC=dscr("s_ropeC", [128, T], F32), ropeS=dscr("s_ropeS", [128, T], F32),
        qTg=dscr("s_qTg", [4, 128, T], BF16), kTg=dscr("s_kTg", [4, 128, T], BF16),
        laT=dscr("s_laT", [2, 4, 128, T], F32),
        vg=dscr("s_vg", [T, 1024], BF16), gaT=dscr("s_gaT", [1024, T], BF16), sgbT=dscr("s_sgbT", [1024, T], BF16),
        qnT=dscr("s_qnT", [8, 128, T], BF16), qx1=dscr("s_qx1", [2, 128, T], BF16), qx2=dscr("s_qx2", [2, 128, T], BF16),
        knT=dscr("s_knT", [8, 128, T], BF16), krT=dscr("s_krT", [32, T], BF16),
        vm=dscr("s_vm", [T, 1040], BF16),
        maT=dscr("s_maT", [1024, T], BF16), mbT=dscr("s_mbT", [1024, T], BF16),
        h1T=dscr("s_h1T", [1024, T], BF16), gatesT=dscr("s_gatesT", [32, T], BF16),
        yacc=dscr("s_yacc", [T, D], F32),
    )

    with ExitStack() as es:
        P = Prog(nc, es)

        def sb(name, shape, dt, stack=es):
            return stack.enter_context(nc.sbuf_tensor(name, list(shape), dt))

        def ps(name, shape, dt, stack=es):
            return stack.enter_context(nc.psum_tensor(name, list(shape), dt))

        ident_f = sb("ident_f", [128, 128], F32)
        ident_b = sb("ident_b", [128, 128], BF16)
        ones_b = sb("ones_b", [128, 512], BF16)
        ones_f = sb("ones_f", [128, 128], F32)
        P.dma("sp", ident_f[:], I["c_ident"], w=["ident_f"])
        P.cp("dve", ident_b[:], ident_f[:], r=["ident_f"], w=["ident_b"])
        P.memset("pool", ones_b[:], 1.0, w=["ones_b"])
        P.memset("pool", ones_f[:], 1.0, w=["ones_f"])
        P.barrier()

        def ln_rows(Pq, xt, g_bc, b_bc, tmp, stat, mv, keys_in, key_out, out_t):
            for c in range(2):
                Pq.add("dve", lambda e, c=c: e.bn_stats(out=stat[:, c, :], in_=xt[:, c * 512:(c + 1) * 512]),
                       r=keys_in, w=[("stat", id(stat))])
            Pq.add("dve", lambda e: e.bn_aggr(out=mv[:, 0:2], in_=stat[:].rearrange("p a b -> p (a b)")),
                   r=[("stat", id(stat))], w=[("mv", id(mv))])
            Pq.ts("dve", mv[:, 2:3], mv[:, 1:2], LN_EPS, ALU.add, r=[("mv", id(mv))], w=[("mv2", id(mv))])
            Pq.act(mv[:, 3:4], mv[:, 2:3], AF.Ln, r=[("mv2", id(mv))], w=[("mv3", id(mv))])
            Pq.act(mv[:, 4:5], mv[:, 3:4], AF.Exp, scale=-0.5, r=[("mv3", id(mv))], w=[("mv4", id(mv))])
            Pq.ts("dve", tmp[:], xt[:], mv[:, 0:1], ALU.subtract, s2=mv[:, 4:5], op1=ALU.mult,
                  r=list(keys_in) + [("mv4", id(mv)), ("mv", id(mv))], w=[("tmp", id(tmp))])
            Pq.tt("pool", tmp[:], tmp[:], g_bc[:], ALU.mult, r=[("tmp", id(tmp)), "lnconst"], w=[("tmp", id(tmp))])
            Pq.tt("pool", out_t[:], tmp[:], b_bc[:], ALU.add, r=[("tmp", id(tmp)), "lnconst"], w=[key_out])

        if "p0" in phases:
            with ExitStack() as st:
                g_bc = sb("p0_g", [128, D], F32, st)
                b_bc = sb("p0_b", [128, D], F32, st)
                inv = sb("p0_inv", [128, 1], F32, st)
                P.dma("sp", g_bc[:], I["ln_emb_g"].partition_broadcast(128), w=["lnconst"])
                P.dma("sp", b_bc[:], I["ln_emb_b"].partition_broadcast(128), w=["lnconst"])
                P.dma("sp", inv[:], I["c_inv"], w=["inv"])
                NB = 3
                xt = [sb("p0_x%d" % i, [128, D], F32, st) for i in range(NB)]
                tmp = [sb("p0_t%d" % i, [128, D], F32, st) for i in range(NB)]
                ot = [sb("p0_o%d" % i, [128, D], F32, st) for i in range(NB)]
                stat = [sb("p0_s%d" % i, [128, 2, 6], F32, st) for i in range(NB)]
                mv = [sb("p0_m%d" % i, [128, 8], F32, st) for i in range(NB)]
                for i in range(T // 128):
                    b = i % NB
                    P.dma("sp", xt[b][:], I["x"][i * 128:(i + 1) * 128, :], w=[("x", b)])
                    ln_rows(P, xt[b], g_bc, b_bc, tmp[b], stat[b], mv[b], [("x", b)], ("o", b), ot[b])
                    P.dma("pool", SC["h"][i * 128:(i + 1) * 128, :], ot[b][:], r=[("o", b)])
                pi_ = sb("p0_pi", [128, 512], I32, st)
                pf = sb("p0_pf", [128, 512], F32, st)
                a1 = sb("p0_a1", [128, 512], F32, st)
                a2 = sb("p0_a2", [128, 512], F32, st)
                kf = sb("p0_kf", [128, 512], F32, st)
                for g in range(NG):
                    P.dma("sp", pi_[:], I["pos"][g * 512:(g + 1) * 512].partition_broadcast(128), w=["pi"])
                    P.cp("dve", pf[:], pi_[:], r=["pi"], w=["pf"])
                    P.ts("dve", pf[:], pf[:], inv[:, 0:1], ALU.mult, r=["pf", "inv"], w=["pf"])
                    for (dst, shift, a) in ((SC["ropeS"], 0.0, a1), (SC["ropeC"], 0.25, a2)):
                        kk = "ang%d" % int(shift > 0)
                        if shift:
                            P.ts("dve", a[:], pf[:], shift, ALU.add, r=["pf"], w=[kk])
                            src = a
                        else:
                            src = pf
                        P.cp("dve", pi_[:], src[:], r=["pf", kk], w=["pi"])
                        P.cp("dve", kf[:], pi_[:], r=["pi"], w=["kf"])
                        P.tt("dve", a[:], src[:], kf[:], ALU.subtract, r=["pf", kk, "kf"], w=[kk])
                        P.ts("dve", kf[:], a[:], 0.5, ALU.is_gt, r=[kk], w=["kf"])
                        P.tt("dve", a[:], a[:], kf[:], ALU.subtract, r=[kk, "kf"], w=[kk])
                        P.ts("dve", kf[:], a[:], -0.5, ALU.is_lt, r=[kk], w=["kf"])
                        P.tt("dve", a[:], a[:], kf[:], ALU.add, r=[kk, "kf"], w=[kk])
                        P.act(a[:], a[:], AF.Sin, scale=TWO_PI, r=[kk], w=[kk])
                        P.dma("pool", dst[:, g * 512:(g + 1) * 512], a[:], r=[kk])
                P.barrier()
                P.flush()

        finish(nc, P, I, SC, out, S, NS, T, NG, sb, ps, ident_f, ident_b, ones_b, ones_f, phases, depth, debug)
    return nc


class Ctx:
    pass


def finish(nc, P, I, SC, out, S, NS, T, NG, sb, ps, ident_f, ident_b, ones_b, ones_f, phases, depth, debug):
    C = Ctx()
    C.nc, C.P, C.I, C.SC, C.out, C.S, C.NS, C.T, C.NG = nc, P, I, SC, out, S, NS, T, NG
    uid = [0]

    def sbu(name, shape, dt, stack):
        uid[0] += 1
        return sb("%s_u%d" % (name, uid[0]), shape, dt, stack)

    def psu(name, shape, dt, stack):
        uid[0] += 1
        return ps("%s_u%d" % (name, uid[0]), shape, dt, stack)

    C.sb, C.ps, C.ident_f, C.ident_b, C.ones_b, C.ones_f = sbu, psu, ident_f, ident_b, ones_b, ones_f
    C.debug = debug
    C.stq_i = 0
    for l in range(depth):
        if "p1" in phases:
            phase_p1(C, l)
        if "gla" in phases:
            phase_gla(C, l)
        if "mla" in phases:
            phase_mla(C, l)
        if "p3" in phases:
            phase_p3(C, l)
        if "moe" in phases:
            phase_moe(C, l, last=(l == depth - 1))
    P.barrier()
    P.flush()


def stq(C):
    C.stq_i ^= 1
    return "sp" if C.stq_i else "pool"


def col_ap(vec_ap):
    return vec_ap.rearrange("(p o) -> p o", o=1)


def phase_p1(C, l):
    P, nc, I, SC, T, NG = C.P, C.nc, C.I, C.SC, C.T, C.NG
    ident_b, ones_b, ones_f = C.ident_b, C.ones_b, C.ones_f
    FM = []
    for h in range(4):
        FM.append(("gq", h, O_GQ + 128 * h, 128))
    for h in range(4):
        FM.append(("gk", h, O_GK + 128 * h, 128))
    FM.append(("zf", 0, O_ZF, 16))
    FM.append(("zb", 1, O_ZB, 16))
    for c in range(3):
        FM.append(("cq", c, O_CQ + 128 * c, 128))
    FM.append(("ckv", 0, O_CKV, 128))
    FM.append(("kr1", 0, O_KR, 16))
    FM.append(("kr2", 0, O_KR + 16, 16))
    for c in range(8):
        FM.append(("gr", c, O_GR + 128 * c, 128))
        FM.append(("ga", c, O_GA + 128 * c, 128))
    for c in range(8):
        FM.append(("gb", c, O_GB + 128 * c, 128))
    with ExitStack() as st:
        def sb(n, shp, dt, stack=st):
            return C.sb("p1_" + n, shp, dt, stack)

        w_in = sb("w_in", [128, 8, D_IN], BF16)
        bcol = sb("bcol", [128, len(FM)], F32)
        gvb = sb("gvb", [1, 1024], BF16)
        wa2 = sb("wa2", [16, 2, 512], BF16)
        nba = sb("nba", [128, 8], F32)
        gnorm = sb("gnorm", [128, 8], F32)
        qg = sb("qg", [128, 4], F32)
        epsc = sb("epsc", [128, 1], F32)
        w_uq = sb("w_uq", [128, 3, 1536], BF16)
        w_kk = sb("w_kk", [128, 1024], BF16)
        w_vv = sb("w_vv", [128, 16, 65], BF16)
        vbias = sb("vbias", [1, 1040], BF16)
        for k in range(8):
            P.dma("pool", w_in[:, k, :], I["w_in"][l, k * 128:(k + 1) * 128, :], w=["w_in"])
        for ci, (_, _, off, M) in enumerate(FM):
            P.dma("sp", bcol[0:M, ci:ci + 1], col_ap(I["b_in"][l, off:off + M]), w=["bcol"])
        P.dma("pool", gvb[0:1, :], I["b_in"][l:l + 1, O_GV:O_GV + 1024], w=["gvb"])
        P.dma("pool", wa2[:, 0, :], I["wa2_f"][l], w=["wa2"])
        P.dma("pool", wa2[:, 1, :], I["wa2_b"][l], w=["wa2"])
        for d_, nm in enumerate(("ba_f", "ba_b")):
            for h in range(4):
                P.dma("sp", nba[:, d_ * 4 + h:d_ * 4 + h + 1], col_ap(I[nm][l, h * 128:(h + 1) * 128]), w=["nba"])
        P.ts("pool", nba[:], nba[:], -1.0, ALU.mult, r=["nba"], w=["nba"])
        for c in range(8):
            P.dma("sp", gnorm[:, c:c + 1], col_ap(I["gla_norm_g"][l, c * 128:(c + 1) * 128]), w=["gnorm"])
        for k in range(3):
            P.dma("sp", qg[:, k:k + 1], col_ap(I["q_norm_g"][l, k * 128:(k + 1) * 128]), w=["qg"])
        P.dma("sp", qg[:, 3:4], col_ap(I["kv_norm_g"][l, :]), w=["qg"])
        P.memset("pool", epsc[:], RMS_EPS, w=["epsc"])
        P.dma("pool", vbias[0:1, :], I["c_vbias"], w=["vbias"])
        with ExitStack() as st2:
            s_uq = sb("s_uq", [128, 3, 1536], F32, st2)
            s_kv = sb("s_kv", [128, 2048], F32, st2)
            P.dma("sp", s_uq[:], I["w_uq"][l].rearrange("(k p) n -> p k n", p=128), w=["s_uq"])
            P.dma("sp", s_kv[:], I["w_ukv"][l], w=["s_kv"])
            for k in range(3):
                P.ts("dve", w_uq[:, k, :], s_uq[:, k, :], qg[:, k:k + 1], ALU.mult, r=["s_uq", "qg"], w=["w_uq"])
            P.ts("dve", w_kk[:], s_kv[:, 0:1024], qg[:, 3:4], ALU.mult, r=["s_kv", "qg"], w=["w_kk"])
            P.memset("pool", w_vv[:], 0.0, w=["w_vv"])
            P.ts("dve", w_vv[:, :, 0:64], s_kv[:, 1024:2048].rearrange("p (h d) -> p h d", d=64), qg[:, 3:4], ALU.mult,
                 r=["s_kv", "qg"], w=["w_vv"])
            P.barrier()
        w_vvf = w_vv[:].rearrange("p h d -> p (h d)")
        ht = [sb("ht%d" % i, [128, D], F32) for i in range(2)]
        hb = [sb("hb%d" % i, [128, D], BF16) for i in range(2)]
        hT4 = [sb("hT4%d" % i, [128, 8, 512], BF16) for i in range(1)] * 2
        cosT = [sb("cos%d" % i, [128, 512], F32) for i in range(2)]
        sinT = [sb("sin%d" % i, [128, 512], F32) for i in range(2)]
        NFP, NBP = 8, 12
        fpl = [sb("fp%d" % i, [128, 512], F32) for i in range(NFP)]
        bpl = [sb("bp%d" % i, [128, 512], BF16) for i in range(NBP)]
        cq_sb = sb("cq_sb", [128, 3, 512], F32)
        cqn = sb("cqn", [128, 3, 512], BF16)
        ckvn = sb("ckvn", [128, 512], BF16)
        vt = [sb("vt%d" % i, [128, 1040], BF16) for i in range(2)]
        gvt = [sb("gvt%d" % i, [128, 1024], BF16) for i in range(2)]
        zt = [sb("zt%d" % i, [16, 512], BF16) for i in range(2)]
        pst = C.ps("p1_pst", [128, 8, 128], BF16, st)
        NPP = 7
        ppl = [C.ps("p1_pp%d" % i, [128, 512], F32, st) for i in range(NPP)]
        cnt = {"fp": 0, "bp": 0, "pp": 0, "ev": 0}

        def fp():
            i = cnt["fp"] % NFP
            cnt["fp"] += 1
            return fpl[i], ("fp", i)

        def bp():
            i = cnt["bp"] % NBP
            cnt["bp"] += 1
            return bpl[i], ("bp", i)

        def pp():
            i = cnt["pp"] % NPP
            cnt["pp"] += 1
            return ppl[i], ("pp", i)

        def evac_copy(dst, src, r, w):
            cnt["ev"] += 1
            P.cp("act" if cnt["ev"] % 2 else "dve", dst, src, r=r, w=w)

        def rope(f1, k1, f2, k2, cos_, sin_, ck, M, dst1, dst2):
            a, ka = fp()
            b, kb = fp()
            P.tt("pool", a[0:M, :], f1[0:M, :], cos_[0:M, :], ALU.mult, r=[k1, ck], w=[ka])
            P.tt("pool", b[0:M, :], f2[0:M, :], sin_[0:M, :], ALU.mult, r=[k2, ck], w=[kb])
            r1, kr1 = bp()
            P.tt("dve", r1[0:M, :], a[0:M, :], b[0:M, :], ALU.subtract, r=[ka, kb], w=[kr1])
            P.dma(stq(C), dst1, r1[0:M, :], r=[kr1])
            a, ka = fp()
            b, kb = fp()
            P.tt("pool", a[0:M, :], f1[0:M, :], sin_[0:M, :], ALU.mult, r=[k1, ck], w=[ka])
            P.tt("pool", b[0:M, :], f2[0:M, :], cos_[0:M, :], ALU.mult, r=[k2, ck], w=[kb])
            r2, kr2 = bp()
            P.tt("dve", r2[0:M, :], a[0:M, :], b[0:M, :], ALU.add, r=[ka, kb], w=[kr2])
            P.dma(stq(C), dst2, r2[0:M, :], r=[kr2])

        def rms_rstd(sq_list, nfeat):
            pss, kp = pp()
            for i, (sq, ksq) in enumerate(sq_list):
                P.mm(pss[:], ones_b[:, 0:128], sq[:], start=(i == 0), stop=(i == len(sq_list) - 1), r=[ksq, "ones_b"], w=[kp])
            t1, kt1 = fp()
            P.act(t1[:], pss[:], AF.Ln, bias=epsc[:, 0:1], scale=1.0 / nfeat, r=[kp, "epsc"], w=[kt1])
            t2, kt2 = fp()
            P.act(t2[:], t1[:], AF.Exp, scale=-0.5, r=[kt1], w=[kt2])
            return t2, kt2

        for g in range(NG):
            t0 = g * 512
            gb_ = g % 2
            tsl = slice(t0, t0 + 512)
            ck = ("cs", gb_)
            P.dma("sp", cosT[gb_][:], SC["ropeC"][:, tsl], w=[ck])
            P.dma("sp", sinT[gb_][:], SC["ropeS"][:, tsl], w=[ck])
            kh4 = ("hT4", gb_)
            for j in range(4):
                jb = j % 2
                P.dma("sp", ht[jb][:], SC["h"][t0 + j * 128:t0 + (j + 1) * 128, :], w=[("ht", jb)])
                P.cp("act", hb[jb][:], ht[jb][:], r=[("ht", jb)], w=[("hb", jb)])
                for k in range(8):
                    P.tr(pst[:, k, :], hb[jb][:, k * 128:(k + 1) * 128], ident_b[:], r=[("hb", jb), "ident_b"], w=["pst"])
                P.cp("dve", hT4[gb_][:, :, j * 128:(j + 1) * 128], pst[:], r=["pst"], w=[kh4])
            h4 = hT4[gb_]
            hold = {}
            for ci, (kind, idx, off, M) in enumerate(FM):
                pt, kp = pp()
                for k in range(8):
                    P.mm(pt[0:M, :], w_in[:, k, off:off + M], h4[:, k, :], start=(k == 0), stop=(k == 7), r=["w_in", kh4], w=[kp])
                bc = bcol[0:M, ci:ci + 1]
                if kind == "gq":
                    o, ko = bp()
                    P.ts("dve", o[:], pt[:], bc, ALU.add, s2=128 ** -0.5, op1=ALU.mult, r=[kp, "bcol"], w=[ko])
                    P.dma(stq(C), SC["qTg"][idx, :, tsl], o[:], r=[ko])
                elif kind == "gk":
                    o, ko = bp()
                    P.ts("dve", o[:], pt[:], bc, ALU.add, r=[kp, "bcol"], w=[ko])
                    P.dma(stq(C), SC["kTg"][idx, :, tsl], o[:], r=[ko])
                elif kind in ("zf", "zb"):
                    z = zt[idx]
                    kz = ("zt", idx)
                    P.ts("dve", z[:], pt[0:16, :], bc, ALU.add, r=[kp, "bcol"], w=[kz])
                    for h in range(4):
                        p2, kp2 = pp()
                        P.mm(p2[:], wa2[0:16, idx, h * 128:(h + 1) * 128], z[0:16, :], r=["wa2", kz], w=[kp2])
                        e1, ke1 = fp()
                        P.act(e1[:], p2[:], AF.Exp, bias=nba[:, idx * 4 + h:idx * 4 + h + 1], scale=-1.0, r=[kp2, "nba"], w=[ke1])
                        e2, ke2 = fp()
                        P.act(e2[:], e1[:], AF.Ln, bias=ones_f[:, 0:1], r=[ke1, "ones_f"], w=[ke2])
                        P.dma(stq(C), SC["laT"][idx, h, :, tsl], e2[:], r=[ke2])
                elif kind == "cq":
                    P.ts("dve", cq_sb[:, idx, :], pt[:], bc, ALU.add, r=[kp, "bcol"], w=[("cq_sb", idx)])
                    sq, ksq = bp()
                    P.act(sq[:], cq_sb[:, idx, :], AF.Square, r=[("cq_sb", idx)], w=[ksq])
                    hold.setdefault("cqsq", []).append((sq, ksq))
                    if idx == 2:
                        rstd, kr = rms_rstd(hold["cqsq"], 384)
                        for c in range(3):
                            P.tt("dve", cqn[:, c, :], cq_sb[:, c, :], rstd[:], ALU.mult, r=[("cq_sb", c), kr], w=["cqn"])
                        for j in range(8):
                            p2, kp2 = pp()
                            for k in range(3):
                                P.mm(p2[:], w_uq[:, k, j * 128:(j + 1) * 128], cqn[:, k, :], start=(k == 0), stop=(k == 2), r=["w_uq", "cqn"], w=[kp2])
                            o, ko = bp()
                            evac_copy(o[:], p2[:], [kp2], [ko])
                            P.dma(stq(C), SC["qnT"][j, :, tsl], o[:], r=[ko])
                        for c in range(2):
                            fs = []
                            for part in range(2):
                                p2, kp2 = pp()
                                co = 1024 + part * 256 + c * 128
                                for k in range(3):
                                    P.mm(p2[:], w_uq[:, k, co:co + 128], cqn[:, k, :], start=(k == 0), stop=(k == 2), r=["w_uq", "cqn"], w=[kp2])
                                f, kf = fp()
                                P.cp("act", f[:], p2[:], r=[kp2], w=[kf])
                                fs.append((f, kf))
                            rope(fs[0][0], fs[0][1], fs[1][0], fs[1][1], cosT[gb_], sinT[gb_], ck, 128,
                                 SC["qx1"][c, :, tsl], SC["qx2"][c, :, tsl])
                elif kind == "ckv":
                    cs, kcs = fp()
                    P.ts("dve", cs[:], pt[:], bc, ALU.add, r=[kp, "bcol"], w=[kcs])
                    sq, ksq = bp()
                    P.act(sq[:], cs[:], AF.Square, r=[kcs], w=[ksq])
                    rstd, kr = rms_rstd([(sq, ksq)], 128)
                    P.tt("dve", ckvn[:], cs[:], rstd[:], ALU.mult, r=[kcs, kr], w=["ckvn"])
                    for j in range(8):
                        p2, kp2 = pp()
                        P.mm(p2[:], w_kk[:, j * 128:(j + 1) * 128], ckvn[:], r=["w_kk", "ckvn"], w=[kp2])
                        o, ko = bp()
                        evac_copy(o[:], p2[:], [kp2], [ko])
                        P.dma(stq(C), SC["knT"][j, :, tsl], o[:], r=[ko])
                    for j in range(4):
                        jb = j % 2
                        for (c0, c1) in ((0, 455), (455, 910), (910, 1040)):
                            p2, kp2 = pp()
                            wd = c1 - c0
                            P.mm(p2[:, 0:wd], ckvn[:, j * 128:(j + 1) * 128], w_vvf[:, c0:c1], start=True, stop=False, r=["ckvn", "w_vv"], w=[kp2])
                            P.mm(p2[:, 0:wd], ones_b[0:1, 0:128], vbias[0:1, c0:c1], start=False, stop=True, r=["ones_b", "vbias"], w=[kp2])
                            evac_copy(vt[jb][:, c0:c1], p2[:, 0:wd], [kp2], [("vt", jb)])
                        P.dma(stq(C), SC["vm"][t0 + j * 128:t0 + (j + 1) * 128, :], vt[jb][:], r=[("vt", jb)])
                elif kind in ("kr1", "kr2"):
                    f, kf = fp()
                    P.ts("dve", f[0:16, :], pt[0:16, :], bc, ALU.add, r=[kp, "bcol"], w=[kf])
                    hold[kind] = (f, kf)
                    if kind == "kr2":
                        rope(hold["kr1"][0], hold["kr1"][1], f, kf, cosT[gb_], sinT[gb_], ck, 16,
                             SC["krT"][0:16, tsl], SC["krT"][16:32, tsl])
                elif kind == "gr":
                    o, ko = bp()
                    P.act(o[:], pt[:], AF.Silu, bias=bc, r=[kp, "bcol"], w=[ko])
                    hold["gr"] = (o, ko)
                elif kind == "ga":
                    o, ko = bp()
                    P.act(o[:], pt[:], AF.Sigmoid, bias=bc, r=[kp, "bcol"], w=[ko])
                    o2, ko2 = bp()
                    P.stt(o2[:], hold["gr"][0][:], gnorm[:, idx:idx + 1], o[:], ALU.mult, ALU.mult, r=[hold["gr"][1], ko, "gnorm"], w=[ko2])
                    P.dma(stq(C), SC["gaT"][idx * 128:(idx + 1) * 128, tsl], o2[:], r=[ko2])
                elif kind == "gb":
                    o, ko = bp()
                    P.act(o[:], pt[:], AF.Sigmoid, bias=bc, r=[kp, "bcol"], w=[ko])
                    P.dma(stq(C), SC["sgbT"][idx * 128:(idx + 1) * 128, tsl], o[:], r=[ko])
            for j in range(4):
                jb = j % 2
                for n in range(2):
                    p2, kp2 = pp()
                    for k in range(8):
                        P.mm(p2[:], h4[:, k, j * 128:(j + 1) * 128], w_in[:, k, O_GV + n * 512:O_GV + (n + 1) * 512],
                             start=(k == 0), stop=False, r=[kh4, "w_in"], w=[kp2])
                    P.mm(p2[:], ones_b[0:1, 0:128], gvb[0:1, n * 512:(n + 1) * 512], start=False, stop=True, r=["ones_b", "gvb"], w=[kp2])
                    evac_copy(gvt[jb][:, n * 512:(n + 1) * 512], p2[:], [kp2], [("gvt", jb)])
                P.dma(stq(C), SC["vg"][t0 + j * 128:t0 + (j + 1) * 128, :], gvt[jb][:], r=[("gvt", jb)])
        P.barrier()
        with nc.named_scope("p1_l%d" % l):
            P.flush()


def phase_mla(C, l):
    P, nc, I, SC, S, NS = C.P, C.nc, C.I, C.SC, C.S, C.NS
    ones_f = C.ones_f
    NKB = S // 128
    NQ = S // 512
    GW = 2
    NSB = 3
    groups = [list(range(a, min(a + GW, NKB))) for a in range(0, NKB, GW)]
    with ExitStack() as st:
        def sb(n, shp, dt):
            return C.sb("ml_" + n, shp, dt, st)
        V = sb("V", [128, NKB, 1040], BF16)
        kT = [sb("kT%d" % i, [96, S], BF16) for i in range(2)]
        qT = [sb("qT%d" % i, [96, S], BF16) for i in range(2)]
        PT = [sb("PT%d" % i, [128, GW, 512], BF16) for i in range(NSB)]
        OT = [sb("OT%d" % i, [65, 512], F32) for i in range(2)]
        rinv = [sb("rinv%d" % i, [64, 512], F32) for i in range(2)]
        o1 = [sb("o1%d" % i, [64, 512], F32) for i in range(2)]
        sg = [sb("sg%d" % i, [64, 512], BF16) for i in range(2)]
        ob = [sb("ob%d" % i, [64, 512], BF16) for i in range(2)]
        sc = [C.ps("ml_sc%d" % i, [128, GW, 512], F32, st) for i in range(NSB)]
        otp = C.ps("ml_ot", [65, 512], F32, st)
        rbp = C.ps("ml_rb", [64, 512], F32, st)
        heads = [(s, h) for s in range(NS) for h in range(16)]

        def load_head(idx):
            s, h = heads[idx]
            tb = s * S
            hb_ = idx % 2
            kk, kq = ("kT", hb_), ("qT", hb_)
            P.dma("sp", kT[hb_][0:64, :], SC["knT"][h // 2, (h % 2) * 64:(h % 2) * 64 + 64, tb:tb + S], w=[kk])
            P.dma("sp", kT[hb_][64:96, :], SC["krT"][:, tb:tb + S], w=[kk])
            P.dma("sp", qT[hb_][0:64, :], SC["qnT"][h // 2, (h % 2) * 64:(h % 2) * 64 + 64, tb:tb + S], w=[kq])
            P.dma("sp", qT[hb_][64:80, :], SC["qx1"][h // 8, (h % 8) * 16:(h % 8) * 16 + 16, tb:tb + S], w=[kq])
            P.dma("sp", qT[hb_][80:96, :], SC["qx2"][h // 8, (h % 8) * 16:(h % 8) * 16 + 16, tb:tb + S], w=[kq])

        stream = []
        for idx, (s, h) in enumerate(heads):
            for qi in range(NQ):
                for gi, grp in enumerate(groups):
                    stream.append((idx, s, h, qi, gi, grp))

        def S_(n):
            idx, s, h, qi, gi, grp = stream[n]
            hb_ = idx % 2
            b = n % NSB
            if qi == 0 and gi == 0:
                if idx == 0:
                    load_head(0)
                if idx + 1 < len(heads):
                    load_head(idx + 1)
            qsl = slice(qi * 512, (qi + 1) * 512)
            for j, kb in enumerate(grp):
                P.mm(sc[b][:, j, :], kT[hb_][:, kb * 128:(kb + 1) * 128], qT[hb_][:, qsl], r=[("kT", hb_), ("qT", hb_)], w=[("sc", b)])

        def epi1(s, h, qi, ib):
            tb = s * S
            P.dma("sp", sg[ib][:], SC["sgbT"][h * 64:(h + 1) * 64, tb + qi * 512:tb + (qi + 1) * 512], w=[("sg", ib)])
            P.cp("dve", OT[ib][:], otp[:], r=["otp"], w=[("OT", ib)])

        def epi2(s, h, qi, ib):
            tb = s * S
            P.mm(rbp[:], ones_f[64:65, 0:64], OT[ib][64:65, :], r=["ones_f", ("OT", ib)], w=["rbp"])
            P.add("dve", lambda e, ib=ib: e.reciprocal(out=rinv[ib][:], in_=rbp[:]), r=["rbp"], w=[("rinv", ib)])
            P.tt("dve", o1[ib][:], OT[ib][0:64, :], rinv[ib][:], ALU.mult, r=[("OT", ib), ("rinv", ib)], w=[("o1", ib)])
            P.tt("pool", ob[ib][:], o1[ib][:], sg[ib][:], ALU.mult, r=[("o1", ib), ("sg", ib)], w=[("ob", ib)])
            P.dma("pool", SC["mbT"][h * 64:(h + 1) * 64, tb + qi * 512:tb + (qi + 1) * 512], ob[ib][:], r=[("ob", ib)])

        NSTR = len(stream)
        for n in range(min(2, NSTR)):
            S_(n)
        pending = None
        it = 0
        for n in range(NSTR):
            idx, s, h, qi, gi, grp = stream[n]
            b = n % NSB
            ng = len(grp)
            if n + 2 < NSTR:
                S_(n + 2)
            if h == 0 and qi == 0 and gi == 0:
                P.dma("sp", V[:], SC["vm"][s * S:(s + 1) * S, :].rearrange("(n p) d -> p n d", p=128), w=["V"])
            P.act(PT[b][:, 0:ng, :], sc[b][:, 0:ng, :], AF.Exp, scale=MLA_SCALE, r=[("sc", b)], w=[("PT", b)])
            for j, kb in enumerate(grp):
                P.mm(otp[:], V[:, kb, h * 65:(h + 1) * 65], PT[b][:, j, :], start=(kb == 0), stop=(kb == NKB - 1),
                     r=["V", ("PT", b)], w=["otp"])
            if pending is not None:
                epi2(*pending)
                pending = None
            if gi == len(groups) - 1:
                ib = it % 2
                it += 1
                epi1(s, h, qi, ib)
                pending = (s, h, qi, ib)
        if pending is not None:
            epi2(*pending)
        P.barrier()
        with nc.named_scope("mla_l%d" % l):
            P.flush()


def phase_gla(C, l):
    P, nc, I, SC, S, NS = C.P, C.nc, C.I, C.SC, C.S, C.NS
    ident_b, ones_f = C.ident_b, C.ones_f
    NCH = S // 128
    W = min(1024, S)
    NBLK = S // W
    CPB = W // 128
    with ExitStack() as st:
        def sb(n, shp, dt):
            return C.sb("gl_" + n, shp, dt, st)
        maskf = sb("maskf", [128, 128], BF16)
        maskb = sb("maskb", [128, 128], BF16)
        scanm = sb("scanm", [128, 1024], F32)
        epsc = sb("epsc", [128, 1], F32)
        P.dma("pool", maskf[:], I["c_maskf"], w=["maskf"])
        P.dma("pool", maskb[:], I["c_maskb"], w=["maskb"])
        P.dma("sp", scanm[:], I["c_scan"], w=["scanm"])
        P.memset("pool", epsc[:], RMS_EPS, w=["epsc"])
        q = sb("q", [128, S], BF16)
        k = sb("k", [128, S], BF16)
        names = ("qdF", "kiF", "keF", "qdB", "kiB", "keB")
        pre = {n: sb(n, [128, S], BF16) for n in names}
        la = [sb("la%d" % i, [128, W], F32) for i in range(2)]
        bb = [sb("bb%d" % i, [128, W], F32) for i in range(2)]
        tmp = [sb("tmp%d" % i, [128, W], F32) for i in range(3)]
        dec = sb("dec", [128, 2, NCH], F32)
        v = sb("v", [128, NCH, 256], BF16)
        stB = sb("stB", [128, NCH, 256], BF16)
        stf = [sb("stf%d" % i, [128, 256], F32) for i in range(2)]
        stFb = [sb("stFb%d" % i, [128, 256], BF16) for i in range(2)]
        kvall = [sb("kvall%d" % i, [128, NCH, 256], F32) for i in range(2)]
        kEt = [sb("kEt%d" % i, [128, 128], BF16) for i in range(2)]
        t12 = [sb("t12%d" % i, [128, 2, 128], BF16) for i in range(2)]
        junk = [sb("junk%d" % i, [128, 256], F32) for i in range(2)]
        ssq = [sb("ssq%d" % i, [128, 4], F32) for i in range(2)]
        on = [sb("on%d" % i, [128, 256], BF16) for i in range(2)]
        gat = [sb("gat%d" % i, [128, 2, 128], BF16) for i in range(2)]
        mat = [sb("mat%d" % i, [128, 2, 128], BF16) for i in range(2)]
        sTp = [C.ps("gl_sT%d" % i, [128, 4, 128], F32, st) for i in range(2)]
        ops_ = [C.ps("gl_o%d" % i, [128, 512], F32, st) for i in range(2)]
        trp = [C.ps("gl_tr%d" % i, [128, 8, 128], BF16, st) for i in range(2)]
        kvp = [C.ps("gl_kv%d" % i, [128, 512], F32, st) for i in range(2)]
        cnt = {"tmp": 0, "i": 0}

        def tm():
            i = cnt["tmp"] % 3
            cnt["tmp"] += 1
            return tmp[i], ("tmp", i)

        for s in range(NS):
            tb = s * S
            for h in range(4):
                P.dma("sp", q[:], SC["qTg"][h, :, tb:tb + S], w=["q"])
                P.dma("sp", k[:], SC["kTg"][h, :, tb:tb + S], w=["k"])
                P.dma("pool", v[:], SC["vg"][tb:tb + S, h * 256:(h + 1) * 256].rearrange("(n p) d -> p n d", p=128), w=["v"])
                for blk in range(NBLK):
                    bs = slice(blk * W, (blk + 1) * W)
                    for d_ in range(2):
                        P.dma("sp", la[d_][:], SC["laT"][d_, h, :, tb + blk * W:tb + (blk + 1) * W], w=[("la", d_)])
                        P.add("dve", lambda e, d_=d_: e.tensor_tensor_scan(out=bb[d_][:], data0=scanm[:, 0:W], data1=la[d_][:],
                                                                           initial=0.0, op0=ALU.mult, op1=ALU.add),
                              r=[("la", d_), "scanm"], w=[("bb", d_)])
                    bF3 = bb[0][:].rearrange("p (c t) -> p c t", t=128)
                    bB3 = bb[1][:].rearrange("p (c t) -> p c t", t=128)
                    totF = bF3[:, :, 127:128]
                    totB = bB3[:, :, 127:128]
                    P.act(dec[:, 0, blk * CPB:(blk + 1) * CPB], bF3[:, :, 127], AF.Exp, scale=-1.0 / 16.0, r=[("bb", 0)], w=["dec"])
                    P.act(dec[:, 1, blk * CPB:(blk + 1) * CPB], bB3[:, :, 127], AF.Exp, scale=-1.0 / 16.0, r=[("bb", 1)], w=["dec"])

                    def emit(dst, src, e_in, kin, scale=1.0):
                        t, kt = tm()
                        P.act(t[:], e_in, AF.Exp, scale=-scale / 16.0, r=kin, w=[kt])
                        P.tt("dve", pre[dst][:, bs], src[:, bs], t[:], ALU.mult, r=[kt, "q", "k"], w=[dst])
                    emit("qdF", q, bb[0][:], [("bb", 0)])
                    emit("kiF", k, bb[0][:], [("bb", 0)], scale=-1.0)
                    t, kt = tm()
                    P.tt("dve", t[:].rearrange("p (c t) -> p c t", t=128), totF.to_broadcast([128, CPB, 128]), bF3, ALU.subtract,
                         r=[("bb", 0)], w=[kt])
                    emit("keF", k, t[:], [kt])
                    t2, kt2 = tm()
                    P.tt("dve", t2[:].rearrange("p (c t) -> p c t", t=128), totB.to_broadcast([128, CPB, 128]), bB3, ALU.subtract,
                         r=[("bb", 1)], w=[kt2])
                    P.tt("dve", t2[:], t2[:], la[1][:], ALU.add, r=[kt2, ("la", 1)], w=[kt2])
                    emit("qdB", q, t2[:], [kt2])
                    emit("kiB", k, t2[:], [kt2], scale=-1.0)
                    t3, kt3 = tm()
                    P.tt("dve", t3[:], bb[1][:], la[1][:], ALU.subtract, r=[("bb", 1), ("la", 1)], w=[kt3])
                    emit("keB", k, t3[:], [kt3])
                P.memset("pool", stf[0][:], 0.0, w=[("stf", 0)])
                curb = [0]

                def kv_step(d_, nm, c):
                    cs = slice(c * 128, (c + 1) * 128)
                    i = cnt["i"] % 2
                    cnt["i"] += 1
                    P.tr(trp[i][:, 0, :], pre[nm][:, cs], ident_b[:], r=[nm, "ident_b"], w=[("trp", i)])
                    P.cp("act", kEt[i][:], trp[i][:, 0, :], r=[("trp", i)], w=[("kEt", i)])
                    P.mm(kvp[i][:, 0:256], kEt[i][:], v[:, c, :], r=[("kEt", i), "v"], w=[("kvp", i)])
                    P.cp("act", kvall[d_][:, c, :], kvp[i][:, 0:256], r=[("kvp", i)], w=[("kvall", d_, c)])

                def bwd_step(c):
                    cur = curb[0]
                    P.cp("act", stB[:, c, :], stf[cur][:], r=[("stf", cur)], w=[("stB", c)])
                    if c == 0:
                        return
                    P.stt(stf[1 - cur][:], stf[cur][:], dec[:, 1, c:c + 1], kvall[1][:, c, :], ALU.mult, ALU.add,
                          r=[("stf", cur), "dec", ("kvall", 1, c)], w=[("stf", 1 - cur)])
                    curb[0] = 1 - cur

                for c in range(NCH):
                    cb = NCH - 1 - c
                    if cb > 0:
                        kv_step(1, "keB", cb)
                    if c < NCH - 1:
                        kv_step(0, "keF", c)
                    if c >= 1:
                        bwd_step(NCH - c)
                bwd_step(0)
                P.memset("pool", stf[0][:], 0.0, w=[("stf", 0)])
                P.memset("pool", stf[1][:], 0.0, w=[("stf", 1)])
                cur = 0
                for c in range(NCH):
                    cs = slice(c * 128, (c + 1) * 128)
                    i = cnt["i"] % 2
                    cnt["i"] += 1
                    tok0 = tb + c * 128
                    P.cp("act", stFb[i][:], stf[cur][:], r=[("stf", cur)], w=[("stFb", i)])
                    if c < NCH - 1:
                        P.stt(stf[1 - cur][:], stf[cur][:], dec[:, 0, c:c + 1], kvall[0][:, c, :], ALU.mult, ALU.add,
                              r=[("stf", cur), "dec", ("kvall", 0, c)], w=[("stf", 1 - cur)])
                        cur = 1 - cur
                    P.dma("sp", gat[i][:], SC["gaT"][h * 256:(h + 1) * 256, tok0:tok0 + 128].rearrange("(a p) t -> p a t", p=128), w=[("gat", i)])
                    P.mm(sTp[i][:, 0, :], pre["kiF"][:, cs], pre["qdF"][:, cs], r=["kiF", "qdF"], w=[("sTp", i)])
                    P.mm(sTp[i][:, 1, :], pre["kiB"][:, cs], pre["qdB"][:, cs], r=["kiB", "qdB"], w=[("sTp", i)])
                    P.tt("dve", t12[i][:, 0, :], sTp[i][:, 0, :], maskf[:], ALU.mult, r=[("sTp", i), "maskf"], w=[("t12", i)])
                    P.tt("dve", t12[i][:, 1, :], sTp[i][:, 1, :], maskb[:], ALU.mult, r=[("sTp", i), "maskb"], w=[("t12", i)])
                    P.mm(ops_[i][:, 0:256], t12[i][:, 0, :], v[:, c, :], start=True, stop=False, r=[("t12", i), "v"], w=[("ops", i)])
                    P.mm(ops_[i][:, 0:256], t12[i][:, 1, :], v[:, c, :], start=False, stop=False, r=[("t12", i), "v"], w=[("ops", i)])
                    P.mm(ops_[i][:, 0:256], pre["qdF"][:, cs], stFb[i][:], start=False, stop=False, r=["qdF", ("stFb", i)], w=[("ops", i)])
                    P.mm(ops_[i][:, 0:256], pre["qdB"][:, cs], stB[:, c, :], start=False, stop=True, r=["qdB", ("stB", c)], w=[("ops", i)])
                    P.act(junk[i][:], ops_[i][:, 0:256], AF.Square, accum=ssq[i][:, 0:1], r=[("ops", i)], w=[("junk", i), ("ssq", i)])
                    P.act(ssq[i][:, 1:2], ssq[i][:, 0:1], AF.Ln, bias=epsc[:, 0:1], scale=1.0 / 256.0, r=[("ssq", i), "epsc"], w=[("ssq1", i)])
                    P.act(ssq[i][:, 2:3], ssq[i][:, 1:2], AF.Exp, scale=-0.5, r=[("ssq1", i)], w=[("ssq2", i)])
                    P.ts("dve", on[i][:], ops_[i][:, 0:256], ssq[i][:, 2:3], ALU.mult, r=[("ops", i), ("ssq2", i)], w=[("on", i)])
                    for a in range(2):
                        P.tr(trp[i][:, 1 + a, :], on[i][:, a * 128:(a + 1) * 128], ident_b[:], r=[("on", i), "ident_b"], w=[("trp", i)])
                    P.tt("dve", mat[i][:], trp[i][:, 1:3, :], gat[i][:], ALU.mult, r=[("trp", i), ("gat", i)], w=[("mat", i)])
                    P.dma("pool", SC["maT"][h * 256:(h + 1) * 256, tok0:tok0 + 128].rearrange("(a p) t -> p a t", p=128), mat[i][:], r=[("mat", i)])
        P.barrier()
        with nc.named_scope("gla_l%d" % l):
            P.flush()


def ln_rows_g(Pq, xt, g_bc, b_bc, tmp, stat, mv, keys_in, key_out, out_t, tag):
    ks, km, kt = ("stat", tag), ("mv", tag), ("lntmp", tag)
    for c in range(2):
        Pq.add("dve", lambda e, c=c: e.bn_stats(out=stat[:, c, :], in_=xt[:, c * 512:(c + 1) * 512]), r=keys_in, w=[ks])
    Pq.add("dve", lambda e: e.bn_aggr(out=mv[:, 0:2], in_=stat[:].rearrange("p a b -> p (a b)")), r=[ks], w=[km])
    Pq.ts("dve", mv[:, 2:3], mv[:, 1:2], LN_EPS, ALU.add, r=[km], w=[km])
    Pq.act(mv[:, 3:4], mv[:, 2:3], AF.Ln, r=[km], w=[km])
    Pq.act(mv[:, 4:5], mv[:, 3:4], AF.Exp, scale=-0.5, r=[km], w=[km])
    Pq.ts("dve", tmp[:], xt[:], mv[:, 0:1], ALU.subtract, s2=mv[:, 4:5], op1=ALU.mult, r=list(keys_in) + [km], w=[kt])
    Pq.tt("dve", tmp[:], tmp[:], g_bc[:], ALU.mult, r=[kt, "lnconst"], w=[kt])
    Pq.tt("dve", out_t[:], tmp[:], b_bc[:], ALU.add, r=[kt, "lnconst"], w=[key_out])


def phase_p3(C, l):
    P, nc, I, SC, T, NG = C.P, C.nc, C.I, C.SC, C.T, C.NG
    ident_f, ones_f = C.ident_f, C.ones_f
    with ExitStack() as st:
        def sb(n, shp, dt):
            return C.sb("p3_" + n, shp, dt, st)
        w_out = sb("w_out", [128, 8, 1024], BF16)
        w_r = sb("w_r", [128, 8, 40], F32)
        b_r = sb("b_r", [1, 40], F32)
        g_bc = sb("g_bc", [128, D], F32)
        b_bc = sb("b_bc", [128, D], F32)
        P.dma("pool", w_out[:], I["w_out"][l].rearrange("(k p) n -> p k n", p=128), w=["w_out"])
        P.dma("sp", w_r[:], I["w_r"][l].rearrange("(k p) n -> p k n", p=128), w=["w_r"])
        P.dma("sp", b_r[0:1, :], I["b_r"][l:l + 1, :], w=["b_r"])
        P.dma("sp", g_bc[:], I["ln1_g"][l].partition_broadcast(128), w=["lnconst"])
        P.dma("sp", b_bc[:], I["ln1_b"][l].partition_broadcast(128), w=["lnconst"])
        ma = [sb("ma%d" % i, [128, 8, 512], BF16) for i in range(2)]
        mb = [sb("mb%d" % i, [128, 8, 512], BF16) for i in range(2)]
        ht = [sb("ht%d" % i, [128, D], F32) for i in range(2)]
        rt = [sb("rt%d" % i, [128, D], F32) for i in range(2)]
        tmp = [sb("tmp%d" % i, [128, D], F32) for i in range(2)]
        h1 = [sb("h1%d" % i, [128, D], F32) for i in range(2)]
        stat = [sb("stat%d" % i, [128, 2, 6], F32) for i in range(2)]
        mv = [sb("mv%d" % i, [128, 8], F32) for i in range(2)]
        hTf = [sb("hTf%d" % i, [128, 8, 128], F32) for i in range(2)]
        hTb = [sb("hTb%d" % i, [128, 8, 128], BF16) for i in range(2)]
        gs = [sb("gs%d" % i, [128, 256], F32) for i in range(2)]
        gTb = [sb("gTb%d" % i, [32, 128], BF16) for i in range(2)]
        mixp = [C.ps("p3_mix%d" % i, [128, 2, 512], F32, st) for i in range(2)]
        pT = C.ps("p3_pT", [128, 8, 128], F32, st)
        rps = C.ps("p3_rps", [128, 512], F32, st)
        gps = C.ps("p3_gps", [128, 512], F32, st)
        def load_group(g):
            t0 = g * 512
            gb_ = g % 2
            km = ("ma", gb_)
            P.dma("sp", ma[gb_][:], SC["maT"][:, t0:t0 + 512].rearrange("(k p) t -> p k t", p=128), w=[km])
            P.dma("sp", mb[gb_][:], SC["mbT"][:, t0:t0 + 512].rearrange("(k p) t -> p k t", p=128), w=[("mb", gb_)])
            P.tt("dve", ma[gb_][:], ma[gb_][:], mb[gb_][:], ALU.add, r=[km, ("mb", gb_)], w=[km])

        def stage_a(n):
            g, j = divmod(n, 4)
            gb_ = g % 2
            km = ("ma", gb_)
            i = n % 2
            tok0 = n * 128
            if j == 0 and g + 1 < NG:
                load_group(g + 1)
                yield
            P.dma("sp", ht[i][:], SC["h"][tok0:tok0 + 128, :], w=[("ht", i)])
            for nn in range(2):
                for k in range(8):
                    P.mm(mixp[i][:, nn, :], ma[gb_][:, k, j * 128:(j + 1) * 128], w_out[:, k, nn * 512:(nn + 1) * 512],
                         start=(k == 0), stop=(k == 7), r=[km, "w_out"], w=[("mixp", i)])
            yield
            P.stt(rt[i][:], ht[i][:], ALPHA, mixp[i][:].rearrange("p a b -> p (a b)"), ALU.mult, ALU.add,
                  r=[("ht", i), ("mixp", i)], w=[("rt", i)])
            yield
            ks, kmv, kt = ("stat", i), ("mv", i), ("lntmp", i)
            for c in range(2):
                P.add("dve", lambda e, c=c, i=i: e.bn_stats(out=stat[i][:, c, :], in_=rt[i][:, c * 512:(c + 1) * 512]), r=[("rt", i)], w=[ks])
                yield
            P.add("dve", lambda e, i=i: e.bn_aggr(out=mv[i][:, 0:2], in_=stat[i][:].rearrange("p a b -> p (a b)")), r=[ks], w=[kmv])
            yield
            P.ts("dve", mv[i][:, 2:3], mv[i][:, 1:2], LN_EPS, ALU.add, r=[kmv], w=[kmv])
            yield
            P.act(mv[i][:, 3:4], mv[i][:, 2:3], AF.Ln, r=[kmv], w=[kmv])
            P.act(mv[i][:, 4:5], mv[i][:, 3:4], AF.Exp, scale=-0.5, r=[kmv], w=[kmv])
            yield
            P.ts("dve", tmp[i][:], rt[i][:], mv[i][:, 0:1], ALU.subtract, s2=mv[i][:, 4:5], op1=ALU.mult, r=[("rt", i), kmv], w=[kt])
            yield
            P.tt("dve", tmp[i][:], tmp[i][:], g_bc[:], ALU.mult, r=[kt, "lnconst"], w=[kt])
            yield
            P.tt("dve", h1[i][:], tmp[i][:], b_bc[:], ALU.add, r=[kt, "lnconst"], w=[("h1", i)])
            yield
            P.dma("pool", SC["h"][tok0:tok0 + 128, :], h1[i][:], r=[("h1", i)], w=[("ht", i)])
            for k in range(8):
                P.tr(pT[:, k, :], h1[i][:, k * 128:(k + 1) * 128], ident_f[:], r=[("h1", i), "ident_f"], w=["pT"])
            P.cp("act", hTf[i][:], pT[:], r=["pT"], w=[("hTf", i)])
            yield
            P.cp("dve", hTb[i][:], hTf[i][:], r=[("hTf", i)], w=[("hTb", i)])
            P.dma("sp", SC["h1T"][:, tok0:tok0 + 128].rearrange("(k p) t -> p k t", p=128), hTb[i][:], r=[("hTb", i)])
            for k in range(8):
                P.mm(rps[:, 0:40], hTf[i][:, k, :], w_r[:, k, :], start=(k == 0), stop=False, r=[("hTf", i), "w_r"], w=["rps"])
            P.mm(rps[:, 0:40], ones_f[0:1, 0:128], b_r[0:1, :], start=False, stop=True, r=["ones_f", "b_r"], w=["rps"])
            yield
            P.cp("act", gs[i][:, 0:40], rps[:, 0:40], r=["rps"], w=[("gs", i)])
            yield

        def stage_b(n):
            i = n % 2
            tok0 = n * 128
            G = gs[i]
            kg = ("gs", i)
            lg, oh, pen, ml, top8 = G[:, 0:40], G[:, 40:48], G[:, 48:56], G[:, 56:88], G[:, 88:96]
            m1, m2, gates = G[:, 96:128], G[:, 128:160], G[:, 160:192]
            scl = lambda a: G[:, 192 + a:193 + a]
            junk8 = G[:, 208:216]
            P.add("dve", lambda e, G=G: e.tensor_reduce(out=G[:, 192:193], in_=G[:, 0:8], axis=AX.X, op=ALU.max), r=[kg], w=[kg])
            yield
            P.ts("dve", oh, lg[:, 0:8], scl(0), ALU.is_equal, r=[kg], w=[kg])
            yield
            P.ts("dve", scl(1), scl(0), -1.0, ALU.mult, r=[kg], w=[kg])
            yield
            P.act(junk8, lg[:, 0:8], AF.Exp, bias=scl(1), accum=scl(2), r=[kg], w=[kg])
            yield
            P.add("dve", lambda e, G=G: e.reciprocal(out=G[:, 195:196], in_=G[:, 194:195]), r=[kg], w=[kg])
            yield
            P.ts("dve", pen, oh, 1.0, ALU.subtract, s2=1e30, op1=ALU.mult, r=[kg], w=[kg])
            yield
            P.tt("dve", ml.rearrange("p (g e) -> p g e", e=4), lg[:, 8:40].rearrange("p (g e) -> p g e", e=4),
                 pen.rearrange("p (g o) -> p g o", o=1).to_broadcast([128, 8, 4]), ALU.add, r=[kg], w=[kg])
            yield
            P.add("dve", lambda e, G=G: e.max(out=G[:, 88:96], in_=G[:, 56:88]), r=[kg], w=[kg])
            yield
            P.ts("dve", m1, ml, top8[:, 0:1], ALU.is_equal, r=[kg], w=[kg])
            yield
            P.ts("dve", m2, ml, top8[:, 1:2], ALU.is_equal, r=[kg], w=[kg])
            yield
            P.tt("dve", scl(4), top8[:, 0:1], top8[:, 1:2], ALU.subtract, r=[kg], w=[kg])
            yield
            P.act(scl(9), scl(4), AF.Exp, scale=-1.0, r=[kg], w=[kg])
            yield
            P.ts("dve", scl(9), scl(9), 1.0, ALU.add, r=[kg], w=[kg])
            yield
            P.add("dve", lambda e, G=G: e.reciprocal(out=G[:, 197:198], in_=G[:, 201:202]), r=[kg], w=[kg])
            yield
            P.ts("dve", scl(6), scl(5), -1.0, ALU.mult, s2=1.0, op1=ALU.add, r=[kg], w=[kg])
            yield
            P.tt("dve", scl(7), scl(5), scl(3), ALU.mult, r=[kg], w=[kg])
            yield
            P.tt("dve", scl(8), scl(6), scl(3), ALU.mult, r=[kg], w=[kg])
            yield
            P.ts("dve", gates, m1, scl(7), ALU.mult, r=[kg], w=[kg])
            yield
            P.stt(gates, m2, scl(8), gates, ALU.mult, ALU.add, r=[kg], w=[kg])
            yield
            P.tr(gps[0:32, 0:128], gates, ident_f[:], r=[kg, "ident_f"], w=["gps"])
            P.cp("act", gTb[i][:], gps[0:32, 0:128], r=["gps"], w=[("gTb", i)])
            P.dma("pool", SC["gatesT"][:, tok0:tok0 + 128], gTb[i][:], r=[("gTb", i)])
            yield

        NTL = NG * 4
        load_group(0)
        for _ in stage_a(0):
            pass
        for n in range(NTL):
            gens = [stage_b(n)]
            if n + 1 < NTL:
                gens.append(stage_a(n + 1))
            while gens:
                for gnr in list(gens):
                    try:
                        next(gnr)
                    except StopIteration:
                        gens.remove(gnr)
        P.barrier()
        with nc.named_scope("p3_l%d" % l):
            P.flush()


def phase_moe(C, l, last):
    P, nc, I, SC, T = C.P, C.nc, C.I, C.SC, C.T
    TT = 512
    NT = T // TT
    GE = 4
    NPASS = NE // GE
    with ExitStack() as st:
        def sb(n, shp, dt):
            return C.sb("mo_" + n, shp, dt, st)
        wg = [sb("wg%d" % i, [128, GE, 8, 256], BF16) for i in range(2)]
        wu = [sb("wu%d" % i, [128, GE, 8, 256], BF16) for i in range(2)]
        wd = [sb("wd%d" % i, [128, GE, 2, 1024], BF16) for i in range(2)]
        sel = sb("sel", [32, NE * 128], BF16)
        g_bc = sb("g_bc", [128, D], F32)
        b_bc = sb("b_bc", [128, D], F32)
        P.dma("pool", sel[:], I["c_sel"], w=["sel"])
        P.dma("sp", g_bc[:], I["ln2_g"][l].partition_broadcast(128), w=["lnconst"])
        P.dma("sp", b_bc[:], I["ln2_b"][l].partition_broadcast(128), w=["lnconst"])
        hT = [sb("hT%d" % i, [128, 8, TT], BF16) for i in range(2)]
        gT = [sb("gT%d" % i, [32, TT], BF16) for i in range(2)]
        Gs = [sb("Gs%d" % i, [128, GE, TT], BF16) for i in range(2)]
        NR = 3
        sgt = [sb("sg%d" % i, [128, TT], BF16) for i in range(NR)]
        tt_ = [sb("tt%d" % i, [128, TT], BF16) for i in range(NR)]
        hid = [sb("hid%d" % i, [128, GE, 2, TT], BF16) for i in range(2)]
        yt = [sb("yt%d" % i, [128, D], F32) for i in range(2)]
        ya = [sb("ya%d" % i, [128, D], F32) for i in range(2)]
        ht = [sb("ht%d" % i, [128, D], F32) for i in range(2)]
        tmp = [sb("tmp%d" % i, [128, D], F32) for i in range(2)]
        ot = [sb("ot%d" % i, [128, D], F32) for i in range(2)]
        stat = [sb("stat%d" % i, [128, 2, 6], F32) for i in range(2)]
        mv = [sb("mv%d" % i, [128, 8], F32) for i in range(2)]
        psy = [C.ps("mo_y%d" % i, [128, 2, 512], F32, st) for i in range(2)]
        psh = [C.ps("mo_h%d" % i, [128, 512], F32, st) for i in range(4)]
        cnt = {"h": 0, "r": 0, "y": 0}

        def load_w(p):
            wb = p % 2
            for ei in range(GE):
                e = p * GE + ei
                P.dma("pool", wg[wb][:, ei, :, :], I["w_gate"][l, e].rearrange("(k p) f -> p k f", p=128), w=[("wg", wb)])
                P.dma("pool", wu[wb][:, ei, :, :], I["w_up"][l, e].rearrange("(k p) f -> p k f", p=128), w=[("wu", wb)])
                P.dma("pool", wd[wb][:, ei, :, :], I["w_down"][l, e].rearrange("(k p) d -> p k d", p=128), w=[("wd", wb)])

        def nextbank():
            ih = cnt["h"] % 4
            cnt["h"] += 1
            return ih

        def GU(p, t, ei):
            wb = p % 2
            tbf = (p * NT + t) % 2
            tok0 = t * TT
            kh, kgt = ("hT", tbf), ("gT", tbf)
            if ei == 0:
                P.dma("sp", hT[tbf][:], SC["h1T"][:, tok0:tok0 + TT].rearrange("(k p) t -> p k t", p=128), w=[kh])
                P.dma("sp", gT[tbf][:], SC["gatesT"][:, tok0:tok0 + TT], w=[kgt])
                for q in range(GE):
                    ih = nextbank()
                    e = p * GE + q
                    P.mm(psh[ih][:], sel[0:32, e * 128:(e + 1) * 128], gT[tbf][0:32, :], r=["sel", kgt], w=[("psh", ih)])
                    P.cp("act", Gs[tbf][:, q, :], psh[ih][:], r=[("psh", ih)], w=[("Gs", tbf)])
            for fc in range(2):
                ig, iu = nextbank(), nextbank()
                for k in range(8):
                    P.mm(psh[ig][:], wg[wb][:, ei, k, fc * 128:(fc + 1) * 128], hT[tbf][:, k, :], start=(k == 0), stop=(k == 7),
                         r=[("wg", wb), kh], w=[("psh", ig)])
                for k in range(8):
                    P.mm(psh[iu][:], wu[wb][:, ei, k, fc * 128:(fc + 1) * 128], hT[tbf][:, k, :], start=(k == 0), stop=(k == 7),
                         r=[("wu", wb), kh], w=[("psh", iu)])
                ir = cnt["r"] % NR
                cnt["r"] += 1
                P.act(sgt[ir][:], psh[ig][:], AF.Silu, r=[("psh", ig)], w=[("sgt", ir)])
                P.tt("dve", tt_[ir][:], psh[iu][:], sgt[ir][:], ALU.mult, r=[("psh", iu), ("sgt", ir)], w=[("tt", ir)])
                P.tt("pool", hid[tbf][:, ei, fc, :], tt_[ir][:], Gs[tbf][:, ei, :], ALU.mult, r=[("tt", ir), ("Gs", tbf)], w=[("hid", tbf, ei)])

        def DN(p, t, rnd):
            wb = p % 2
            tbf = (p * NT + t) % 2
            tok0 = t * TT + rnd * 256
            for j in range(2):
                c0 = rnd * 256 + j * 128
                for n in range(2):
                    for ei in range(GE):
                        for fc in range(2):
                            P.mm(psy[j][:, n, :], hid[tbf][:, ei, fc, c0:c0 + 128], wd[wb][:, ei, fc, n * 512:(n + 1) * 512],
                                 start=(ei == 0 and fc == 0), stop=(ei == GE - 1 and fc == 1), r=[("hid", tbf, ei), ("wd", wb)], w=[("psy", j)])
            for j in range(2):
                tokj = tok0 + j * 128
                iy = cnt["y"] % 2
                cnt["y"] += 1
                yflat = psy[j][:].rearrange("p a b -> p (a b)")
                if p < NPASS - 1:
                    P.cp("act", yt[iy][:], yflat, r=[("psy", j)], w=[("yt", iy)])
                    if p == 0:
                        P.dma("pool", SC["yacc"][tokj:tokj + 128, :], yt[iy][:], r=[("yt", iy)], w=[("yacc", tokj)])
                    else:
                        P.add("pool", lambda e, tokj=tokj, iy=iy: e.dma_start(out=SC["yacc"][tokj:tokj + 128, :], in_=yt[iy][:], accum_op=ALU.add),
                              r=[("yt", iy)], w=[("yacc", tokj)], dma=True)
                else:
                    P.dma("sp", ya[iy][:], SC["yacc"][tokj:tokj + 128, :], r=[("yacc", tokj)], w=[("ya", iy)])
                    P.tt("dve", yt[iy][:], yflat, ya[iy][:], ALU.add, r=[("psy", j), ("ya", iy)], w=[("yt", iy)])
                    P.dma("sp", ht[iy][:], SC["h"][tokj:tokj + 128, :], w=[("ht", iy)])
                    P.stt(yt[iy][:], ht[iy][:], ALPHA, yt[iy][:], ALU.mult, ALU.add, r=[("ht", iy), ("yt", iy)], w=[("yt", iy)])
                    ln_rows_g(P, yt[iy], g_bc, b_bc, tmp[iy], stat[iy], mv[iy], [("yt", iy)], ("ot", iy), ot[iy], iy)
                    dst = C.out if last else SC["h"]
                    P.dma("sp", dst[tokj:tokj + 128, :], ot[iy][:], r=[("ot", iy)], w=[("ht", iy)])

        load_w(0)
        if NPASS > 1:
            load_w(1)
        pend = []
        for p in range(NPASS):
            for t in range(NT):
                for ei in range(GE):
                    GU(p, t, ei)
                    if pend:
                        pp_, tt2, rnd = pend.pop(0)
                        DN(pp_, tt2, rnd)
                        if not pend and pp_ != p and pp_ + 2 < NPASS:
                            load_w(pp_ + 2)
                while pend:
                    DN(*pend.pop(0))
                pend = [(p, t, 0), (p, t, 1)]
        while pend:
            DN(*pend.pop(0))
        P.barrier()
        with nc.named_scope("moe_l%d" % l):
            P.flush()


def make_consts():
    inv = (10000.0 ** (-np.arange(0, 32, 2, dtype=np.float32) / 32)).astype(np.float32)
    c_inv = (np.tile(inv, 8).reshape(128, 1) / np.float32(2 * np.pi)).astype(np.float32)
    j = np.arange(128)[:, None]
    i = np.arange(128)[None, :]
    c_scan = np.ones((128, 1024), np.float32)
    c_scan[:, ::128] = 0.0
    sel = np.zeros((32, NE * 128), np.float32)
    for e in range(NE):
        sel[e, e * 128:(e + 1) * 128] = 1.0
    vb = np.zeros((1, 1040), np.float32)
    vb[0, 64::65] = 1.0
    return dict(c_inv=c_inv, c_ident=np.eye(128, dtype=np.float32),
                c_maskf=(j <= i).astype(np.float32), c_maskb=(j > i).astype(np.float32),
                c_scan=c_scan, c_sel=sel, c_vbias=vb)


def permute_weights(inp):
    w_uq = np.asarray(inp["mla_w_uq"])
    L = w_uq.shape[0]
    w4 = w_uq.reshape(L, 384, 16, 96)
    w_uq_p = np.concatenate([w4[..., 0:64].reshape(L, 384, 1024), w4[..., 64:80].reshape(L, 384, 256),
                             w4[..., 80:96].reshape(L, 384, 256)], axis=-1)
    w_ukv = np.asarray(inp["mla_w_ukv"]).reshape(L, 128, 16, 128)
    w_ukv_p = np.concatenate([w_ukv[..., 0:64].reshape(L, 128, 1024), w_ukv[..., 64:128].reshape(L, 128, 1024)], axis=-1)
    w_r = np.concatenate([np.asarray(inp["w_grp"]), np.asarray(inp["w_exp"])], axis=-1)
    b_r = np.concatenate([np.asarray(inp["b_grp"]), np.asarray(inp["b_exp"])], axis=-1)
    return dict(w_uq=np.ascontiguousarray(w_uq_p), w_ukv=np.ascontiguousarray(w_ukv_p),
                w_r=np.ascontiguousarray(w_r), b_r=np.ascontiguousarray(b_r))


def make_in_maps(inp, n_cores, NS):
    x = np.asarray(inp["x"])
    pos = np.asarray(inp["positions"])
    B, S, _ = x.shape
    assert B == n_cores * NS
    shared = dict(make_consts())
    shared.update(permute_weights(inp))
    ren = dict(ln_emb_g="ln_emb_g", ln_emb_b="ln_emb_b", w_in="w_in", b_in="b_in", wa2_f="gla_wa2_f", ba_f="gla_ba_f",
               wa2_b="gla_wa2_b", ba_b="gla_ba_b", gla_norm_g="gla_norm_g", q_norm_g="mla_q_norm_g",
               kv_norm_g="mla_kv_norm_g", w_out="w_out", ln1_g="ln1_g", ln1_b="ln1_b", w_gate="w_gate", w_up="w_up",
               w_down="w_down", ln2_g="ln2_g", ln2_b="ln2_b")
    for k, v in ren.items():
        shared[k] = np.ascontiguousarray(np.asarray(inp[v]), dtype=np.float32)
    maps = []
    for c in range(n_cores):
        m = dict(shared)
        m["x"] = np.ascontiguousarray(x[c * NS:(c + 1) * NS].reshape(NS * S, D))
        m["pos"] = np.ascontiguousarray(pos[c * NS:(c + 1) * NS].reshape(NS * S).astype(np.int32))
        maps.append(m)
    return maps


_CACHE = {}


def kernel(**inputs):
    x = np.asarray(inputs["x"])
    B, S, _ = x.shape
    n_cores = 8
    NS = B // n_cores
    key = (S, NS)
    if key not in _CACHE:
        _CACHE[key] = build_program(S, NS)
    nc = _CACHE[key]
    maps = make_in_maps(inputs, n_cores, NS)
    res = run_bass_kernel_spmd(nc, maps, core_ids=list(range(n_cores)))
    outs = [np.asarray(r["out"]).reshape(NS, S, D) for r in res.results]
    return np.concatenate(outs, axis=0).astype(np.float32)
```
